# Optimizing a Trainium2 kernel written in Bass

```python
import math
import jax, jax.numpy as jnp
from jax import lax
import numpy as np

D_MODEL = 1024
BATCH = 4
SEQ = 8192
DEPTH = 4

D_MIX = D_MODEL
FOX_HEADS = 8
FOX_HEAD_DIM = 64
FOX_W = FOX_HEADS * FOX_HEAD_DIM
GDN_HEADS = 4
GDN_DK = 128
GDN_DV = 128
GDN_WK = GDN_HEADS * GDN_DK
GDN_WV = GDN_HEADS * GDN_DV
CONV_K = 4
IN_DIM = 3 * FOX_W + FOX_HEADS + 2 * GDN_WK + GDN_WV + 2 * GDN_HEADS + GDN_WV
Q_BLOCK = 128
GDN_CHUNK = 64
D_FF = 2816
N_EXPERTS = 8
TOP_K = 2
D_FF_EXPERT = 3584
MOE_BLOCK = 512
N_DENSE = (DEPTH + 1) // 2
N_MOE = DEPTH // 2
EPS = 1e-6

kernel_name = "hybrid_fox_gdn_moe_trunk"


def rmsnorm(x, g):
    xf = x.astype(jnp.float32)
    y = xf * lax.rsqrt(jnp.mean(xf * xf, axis=-1, keepdims=True) + EPS)
    return (y * g.astype(jnp.float32)).astype(x.dtype)


def causal_depthwise_conv(x, w):
    c = x.shape[-1]
    return lax.conv_general_dilated(
        x, w[:, None, :].astype(x.dtype), window_strides=(1,), padding=[(CONV_K - 1, 0)],
        dimension_numbers=("NWC", "WIO", "NWC"), feature_group_count=c)


def forgetting_attention(q, k, v, f_logit, f_bias):
    B, T, H, dh = q.shape
    nb = T // Q_BLOCK
    scale = dh ** -0.5
    logf = jax.nn.log_sigmoid(f_logit.astype(jnp.float32) + f_bias.astype(jnp.float32))
    c = jnp.cumsum(logf, axis=1).transpose(0, 2, 1)
    qh = q.astype(jnp.float32).transpose(0, 2, 1, 3)
    kh = k.astype(jnp.float32).transpose(0, 2, 1, 3)
    vh = v.astype(jnp.float32).transpose(0, 2, 1, 3)
    qb = qh.reshape(B, H, nb, Q_BLOCK, dh).transpose(2, 0, 1, 3, 4)
    cb = c.reshape(B, H, nb, Q_BLOCK).transpose(2, 0, 1, 3)
    kpos = jnp.arange(T)

    def block(args):
        qi, ci, i = args
        s = jnp.einsum("bhqd,bhkd->bhqk", qi, kh) * scale + ci[..., None] - c[:, :, None, :]
        qpos = i * Q_BLOCK + jnp.arange(Q_BLOCK)
        s = jnp.where(kpos[None, :] <= qpos[:, None], s, -jnp.inf)
        p = jax.nn.softmax(s, axis=-1)
        return jnp.einsum("bhqk,bhkd->bhqd", p, vh)

    o = lax.map(block, (qb, cb, jnp.arange(nb)))
    o = o.transpose(1, 0, 3, 2, 4).reshape(B, T, H * dh)
    return o.astype(q.dtype)


def l2norm(x):
    xf = x.astype(jnp.float32)
    return xf * lax.rsqrt(jnp.sum(xf * xf, axis=-1, keepdims=True) + EPS)


def gated_delta_rule(q, k, v, g, beta):
    B, T, H, dk = q.shape
    dv = v.shape[-1]
    C = GDN_CHUNK
    N = T // C

    def chunk(t):
        t = jnp.moveaxis(t, 2, 1)
        return t.reshape((B, H, N, C) + t.shape[3:])

    q, k, v, g, beta = chunk(q), chunk(k), chunk(v), chunk(g), chunk(beta)
    g = jnp.cumsum(g, axis=-1)
    kb = k * beta[..., None]
    vb = v * beta[..., None]
    tril = jnp.tril(jnp.ones((C, C), dtype=bool))
    strict = jnp.tril(jnp.ones((C, C), dtype=bool), k=-1)
    diff = g[..., :, None] - g[..., None, :]
    L = jnp.where(tril, jnp.exp(jnp.where(tril, diff, 0.0)), 0.0)
    A = jnp.where(strict, jnp.einsum("bhncd,bhnsd->bhncs", kb, k) * L, 0.0)
    eye = jnp.eye(C, dtype=A.dtype)
    rhs = jnp.concatenate([vb, kb * jnp.exp(g)[..., None]], axis=-1)
    sol = lax.linalg.triangular_solve(A + eye, rhs, left_side=True, lower=True,
                                      unit_diagonal=True)
    u, w = sol[..., :dv], sol[..., dv:]
    intra = jnp.where(tril, jnp.einsum("bhncd,bhnsd->bhncs", q, k) * L, 0.0)

    def step(S, inp):
        qi, ki, ui, wi, gi, ai = inp
        v_new = ui - jnp.einsum("bhcd,bhde->bhce", wi, S)
        o = (jnp.einsum("bhcd,bhde->bhce", qi * jnp.exp(gi)[..., None], S)
             + jnp.einsum("bhcs,bhse->bhce", ai, v_new))
        glast = gi[..., -1]
        S = (S * jnp.exp(glast)[..., None, None]
             + jnp.einsum("bhcd,bhce->bhde", ki * jnp.exp(glast[..., None] - gi)[..., None], v_new))
        return S, o

    xs = tuple(jnp.moveaxis(t, 2, 0) for t in (q, k, u, w, g, intra))
    S0 = jnp.zeros((B, H, dk, dv), jnp.float32)
    _, o = lax.scan(step, S0, xs)
    return o.transpose(1, 0, 3, 2, 4).reshape(B, T, H, dv)


def hybrid_mixer(h, w_in, fox_f_bias, fox_norm_g, conv_w, a_log, dt_bias, gdn_norm_g, w_out):
    B, T, _ = h.shape
    z = jnp.einsum("btd,de->bte", h, w_in)
    sizes = [FOX_W, FOX_W, FOX_W, FOX_HEADS, 2 * GDN_WK + GDN_WV, GDN_HEADS, GDN_HEADS, GDN_WV]
    idx = np.cumsum(sizes)[:-1].tolist()
    fq, fk, fv, ff, gqkv, ga, gb, gz = jnp.split(z, idx, axis=-1)

    hs = (B, T, FOX_HEADS, FOX_HEAD_DIM)
    fo = forgetting_attention(fq.reshape(hs), fk.reshape(hs), fv.reshape(hs), ff, fox_f_bias)
    fo = rmsnorm(fo.reshape(hs), fox_norm_g.reshape(FOX_HEADS, FOX_HEAD_DIM)).reshape(B, T, FOX_W)

    gqkv = jax.nn.silu(causal_depthwise_conv(gqkv, conv_w))
    gq, gk, gv = jnp.split(gqkv, [GDN_WK, 2 * GDN_WK], axis=-1)
    gq = l2norm(gq.reshape(B, T, GDN_HEADS, GDN_DK)) * (GDN_DK ** -0.5)
    gk = l2norm(gk.reshape(B, T, GDN_HEADS, GDN_DK))
    gv = gv.reshape(B, T, GDN_HEADS, GDN_DV).astype(jnp.float32)
    g = -jnp.exp(a_log.astype(jnp.float32)) * jax.nn.softplus(ga.astype(jnp.float32) + dt_bias.astype(jnp.float32))
    beta = jax.nn.sigmoid(gb.astype(jnp.float32))
    go = gated_delta_rule(gq, gk, gv, g, beta)
    gate = jax.nn.silu(gz.reshape(B, T, GDN_HEADS, GDN_DV).astype(jnp.float32))
    go = (rmsnorm(go, gdn_norm_g) * gate).reshape(B, T, GDN_WV).astype(h.dtype)

    mix = jnp.concatenate([fo, go], axis=-1)
    return jnp.einsum("bte,ed->btd", mix, w_out)


def swiglu(h, w1, w3, w2):
    a = jnp.einsum("...d,df->...f", h, w1)
    b = jnp.einsum("...d,df->...f", h, w3)
    return jnp.einsum("...f,fd->...d", jax.nn.silu(a) * b, w2)


def moe_swiglu(h, router_w, w1, w3, w2):
    B, T, D = h.shape
    Ntok = B * T
    xf = h.reshape(Ntok, D)
    logits = jnp.einsum("nd,de->ne", xf, router_w).astype(jnp.float32)
    probs = jax.nn.softmax(logits, axis=-1)
    topv, topi = lax.top_k(probs, TOP_K)
    topv = topv / jnp.sum(topv, axis=-1, keepdims=True)
    A = Ntok * TOP_K
    flat_e = topi.reshape(A)
    flat_w = topv.reshape(A)
    flat_tok = jnp.arange(A, dtype=jnp.int32) // TOP_K
    order = jnp.argsort(flat_e)
    se = flat_e[order]
    counts = jnp.bincount(flat_e, length=N_EXPERTS)
    start = jnp.cumsum(counts) - counts
    padded = ((counts + MOE_BLOCK - 1) // MOE_BLOCK) * MOE_BLOCK
    pend = jnp.cumsum(padded)
    pstart = pend - padded
    dest = pstart[se] + jnp.arange(A) - start[se]
    n_blocks = -(-A // MOE_BLOCK) + N_EXPERTS
    P = n_blocks * MOE_BLOCK
    slot_tok = jnp.zeros((P,), jnp.int32).at[dest].set(flat_tok[order])
    slot_w = jnp.zeros((P,), jnp.float32).at[dest].set(flat_w[order])
    block_e = jnp.clip(jnp.searchsorted(pend, jnp.arange(n_blocks) * MOE_BLOCK, side="right"),
                       0, N_EXPERTS - 1)
    xs = xf[slot_tok].reshape(n_blocks, MOE_BLOCK, D)

    def expert_block(args):
        xb, e = args
        return swiglu(xb, w1[e], w3[e], w2[e])

    ys = lax.map(expert_block, (xs, block_e)).reshape(P, D)
    out = jnp.zeros((Ntok, D), ys.dtype).at[slot_tok].add(ys * slot_w[:, None].astype(ys.dtype))
    return out.reshape(B, T, D)


def setup_inputs(seed: int = 0) -> dict:
    key = jax.random.key(seed)
    ks = jax.random.split(key, 20)
    f32 = jnp.float32
    nrm = lambda k, s, sc: jax.random.normal(k, s, f32) * sc
    res_scale = (2 * DEPTH) ** -0.5
    x = jax.random.normal(ks[0], (BATCH, SEQ, D_MODEL), f32)
    ln1_g = 1.0 + nrm(ks[1], (DEPTH, D_MODEL), 0.02)
    w_in = nrm(ks[2], (DEPTH, D_MODEL, IN_DIM), D_MODEL ** -0.5)
    fox_f_bias = jax.random.uniform(ks[3], (DEPTH, FOX_HEADS), f32, 2.0, 7.0)
    fox_norm_g = 1.0 + nrm(ks[4], (DEPTH, FOX_W), 0.02)
    gdn_conv_w = nrm(ks[5], (DEPTH, CONV_K, 2 * GDN_WK + GDN_WV), CONV_K ** -0.5)
    gdn_a_log = jnp.log(jax.random.uniform(ks[6], (DEPTH, GDN_HEADS), f32, 1.0, 16.0))
    dt = jnp.exp(jax.random.uniform(ks[7], (DEPTH, GDN_HEADS), f32, math.log(1e-3), math.log(1e-1)))
    gdn_dt_bias = dt + jnp.log(-jnp.expm1(-dt))
    gdn_norm_g = 1.0 + nrm(ks[8], (DEPTH, GDN_DV), 0.02)
    w_out = nrm(ks[9], (DEPTH, D_MIX, D_MODEL), D_MIX ** -0.5 * res_scale)
    ln2_g = 1.0 + nrm(ks[10], (DEPTH, D_MODEL), 0.02)
    ffn_w1 = nrm(ks[11], (N_DENSE, D_MODEL, D_FF), D_MODEL ** -0.5)
    ffn_w3 = nrm(ks[12], (N_DENSE, D_MODEL, D_FF), D_MODEL ** -0.5)
    ffn_w2 = nrm(ks[13], (N_DENSE, D_FF, D_MODEL), D_FF ** -0.5 * res_scale)
    router_w = nrm(ks[14], (N_MOE, D_MODEL, N_EXPERTS), D_MODEL ** -0.5)
    exp_w1 = nrm(ks[15], (N_MOE, N_EXPERTS, D_MODEL, D_FF_EXPERT), D_MODEL ** -0.5)
    exp_w3 = nrm(ks[16], (N_MOE, N_EXPERTS, D_MODEL, D_FF_EXPERT), D_MODEL ** -0.5)
    exp_w2 = nrm(ks[17], (N_MOE, N_EXPERTS, D_FF_EXPERT, D_MODEL), D_FF_EXPERT ** -0.5 * res_scale)
    final_g = 1.0 + nrm(ks[18], (D_MODEL,), 0.02)
    return {"x": x, "ln1_g": ln1_g, "w_in": w_in, "fox_f_bias": fox_f_bias,
            "fox_norm_g": fox_norm_g, "gdn_conv_w": gdn_conv_w, "gdn_a_log": gdn_a_log,
            "gdn_dt_bias": gdn_dt_bias, "gdn_norm_g": gdn_norm_g, "w_out": w_out,
            "ln2_g": ln2_g, "ffn_w1": ffn_w1, "ffn_w3": ffn_w3, "ffn_w2": ffn_w2,
            "router_w": router_w, "exp_w1": exp_w1, "exp_w3": exp_w3, "exp_w2": exp_w2,
            "final_g": final_g}


def reference(x, ln1_g, w_in, fox_f_bias, fox_norm_g, gdn_conv_w, gdn_a_log, gdn_dt_bias,
              gdn_norm_g, w_out, ln2_g, ffn_w1, ffn_w3, ffn_w2, router_w, exp_w1, exp_w3,
              exp_w2, final_g):
    for layer in range(DEPTH):
        h = rmsnorm(x, ln1_g[layer])
        x = x + hybrid_mixer(h, w_in[layer], fox_f_bias[layer], fox_norm_g[layer],
                             gdn_conv_w[layer], gdn_a_log[layer], gdn_dt_bias[layer],
                             gdn_norm_g[layer], w_out[layer])
        h = rmsnorm(x, ln2_g[layer])
        j = layer // 2
        if layer % 2 == 0:
            x = x + swiglu(h, ffn_w1[j], ffn_w3[j], ffn_w2[j])
        else:
            x = x + moe_swiglu(h, router_w[j], exp_w1[j], exp_w3[j], exp_w2[j])
    return rmsnorm(x, final_g)
```

```python
from contextlib import ExitStack
import numpy as np
import concourse.bass as bass
import concourse.mybir as mybir

F32 = mybir.dt.float32
BF16 = mybir.dt.bfloat16
AF = mybir.ActivationFunctionType
ALU = mybir.AluOpType
AX = mybir.AxisListType

SAME_ENGINE_SYNC = True


class Dep:
    __slots__ = ("w", "rs", "wneeds", "dsem", "name")

    def __init__(self, name=""):
        self.w = None
        self.rs = []
        self.wneeds = []
        self.dsem = None
        self.name = name


class T:
    def __init__(self, h, name, nsub=1):
        self.h = h
        self.name = name
        self.d = Dep(name)
        self.subs = [Dep(name + str(i)) for i in range(nsub)] if nsub > 1 else None

    def __getitem__(self, idx):
        return self.h[idx]


class Prog:
    ENGS = ["pe", "act", "dve", "pool", "sp"]

    def __init__(self, nc):
        self.nc = nc
        self.items = {e: [] for e in self.ENGS}
        self.cnt = {e: 0 for e in self.ENGS}
        self.known = {e: {} for e in self.ENGS}
        self.dsems = []
        self.dcnt = {}
        self.stack = ExitStack()
        self.final_events = []

    def sb(self, name, shape, dtype, nsub=1):
        h = self.stack.enter_context(self.nc.sbuf_tensor(name, list(shape), dtype))
        return T(h, name, nsub)

    def ps(self, name, shape, dtype=F32, nsub=1):
        h = self.stack.enter_context(self.nc.psum_tensor(name, list(shape), dtype))
        return T(h, name, nsub)

    def _need(self, eng, ev, waits):
        if ev is None:
            return
        key, val = ev
        if key == eng and (eng == "pe" or not SAME_ENGINE_SYNC):
            return
        if self.known[eng].get(key, 0) >= val:
            return
        if waits.get(key, 0) < val:
            waits[key] = val

    def _collect(self, eng, reads, writes):
        waits = {}
        for d in reads:
            self._need(eng, d.w, waits)
        wn = {}
        for d in writes:
            evs = [d.w] + list(d.rs)
            for ev in evs:
                self._need(eng, ev, waits)
        for k, v in waits.items():
            self.known[eng][k] = v
        return waits

    def _commit(self, ev, reads, writes):
        for d in reads:
            d.rs.append(ev)
        for d in writes:
            d.w = ev
            d.rs = []

    @staticmethod
    def _deps(lst):
        out = []
        for x in lst:
            if x is None:
                continue
            if isinstance(x, T):
                out.append(x.d)
            else:
                out.append(x)
        return out

    def op(self, eng, fn, reads=(), writes=()):
        reads = self._deps(reads)
        writes = self._deps(writes)
        waits = self._collect(eng, reads, writes)
        self.cnt[eng] += 1
        ev = (eng, self.cnt[eng])
        self._commit(ev, reads, writes)
        self.items[eng].append((fn, waits, eng, 1))

    def dma(self, q, fn, reads=(), writes=(), semdep=None):
        reads = self._deps(reads)
        writes = self._deps(writes)
        if semdep is None:
            semdep = writes[0] if writes else reads[0]
        elif isinstance(semdep, T):
            semdep = semdep.d
        if semdep.dsem is None:
            semdep.dsem = ("dma", len(self.dsems))
            self.dsems.append(semdep)
            self.dcnt[semdep.dsem] = 0
        key = semdep.dsem
        waits = {}
        for d in reads:
            self._need(q, d.w, waits)
        for d in writes:
            evs = list(d.rs)
            if d.w is not None and not (d.w[0] == key and not d.rs):
                evs.append(d.w)
            for ev in evs + (d.wneeds if (d.w is not None and d.w[0] == key and not d.rs) else []):
                self._need(q, ev, waits)
            if not (d.w is not None and d.w[0] == key and not d.rs):
                d.wneeds = evs
        for k, v in waits.items():
            self.known[q][k] = v
        self.dcnt[key] += 16
        ev = (key, self.dcnt[key])
        self._commit(ev, reads, writes)
        self.items[q].append((fn, waits, key, 16))
        return ev

    def finish(self, eng, events):
        waits = {}
        for ev in events:
            self._need(eng, ev, waits)
        self.items[eng].append((None, waits, None, 0))

    def emit(self):
        nc = self.nc
        with ExitStack() as st:
            sems = {}
            for e in self.ENGS:
                sems[e] = st.enter_context(nc.semaphore("s_" + e))
            for i, d in enumerate(self.dsems):
                sems[d.dsem] = st.enter_context(nc.semaphore("d%d" % i))
            block = st.enter_context(nc.Block())

            def replay(ename):
                def f(eng):
                    for fn, waits, inckey, incv in self.items[ename]:
                        for k, v in waits.items():
                            eng.wait_ge(sems[k], v)
                        if fn is not None:
                            ins = fn(eng)
                            ins.then_inc(sems[inckey], incv)
                return f

            block.tensor(replay("pe"))
            block.scalar(replay("act"))
            block.vector(replay("dve"))
            block.gpsimd(replay("pool"))
            block.sync(replay("sp"))
        self.stack.close()


FOX_W = 512
def wa_cols(hh):
    fq, fk, fv, ff = 0, 512, 1024, 1536
    gq, gk, gv = 1544, 1544 + 512, 1544 + 1024
    ga, gb, gz = 3080, 3084, 3088
    heads = [4 * hh + i for i in range(4)]
    gh = [2 * hh, 2 * hh + 1]
    idx = []
    for base in (fq, fk):
        for h in heads:
            idx += list(range(base + h * 64, base + (h + 1) * 64))
    for g in gh:
        for base in (gq, gk, gv):
            idx += list(range(base + g * 128, base + (g + 1) * 128))
    for g in gh:
        idx += list(range(gz + g * 128, gz + (g + 1) * 128))
    for h in heads:
        idx += list(range(fv + h * 64, fv + (h + 1) * 64))
    idx += [ff + h for h in heads]
    idx += [ga + g for g in gh]
    idx += [gb + g for g in gh]
    assert len(idx) == 1800
    return np.array(idx)

def consts():
    i = np.arange(128)
    ident = np.eye(128, dtype=np.float32)
    tri = (i[:, None] <= i[None, :]).astype(np.float32)
    same = (i[:, None] // 64 == i[None, :] // 64)
    triblk = (tri > 0) & same
    strict = (i[:, None] < i[None, :]) & same
    c = np.stack([ident, tri, triblk.astype(np.float32), strict.astype(np.float32), same.astype(np.float32)], axis=1)
    return np.ascontiguousarray(c.astype(np.float32))

def lay8(g):
    return np.ascontiguousarray(g.reshape(8, 128).T)

def prep_A(xTb, ln1_g, w_in, fox_f_bias, fox_norm_g, conv_w, a_log, dt_bias, gdn_norm_g, hh):
    heads = [4 * hh + i for i in range(4)]
    gh = [2 * hh, 2 * hh + 1]
    cw = np.zeros((128, 24), np.float32)
    for g_i, g in enumerate(gh):
        for m in range(3):
            ch0 = m * 512 + g * 128
            cw[:, (g_i * 3 + m) * 4:(g_i * 3 + m) * 4 + 4] = conv_w[:, ch0:ch0 + 128].T
    return {
        "xT": xTb,
        "WA": np.ascontiguousarray(w_in[:, wa_cols(hh)]),
        "g1": lay8(ln1_g),
        "fbias": np.ascontiguousarray(np.broadcast_to(fox_f_bias[heads][None, :], (128, 4))),
        "gfox": np.ascontiguousarray(fox_norm_g.reshape(8, 64)[heads].T),
        "convw": cw,
        "alog": np.ascontiguousarray(np.broadcast_to(a_log[gh][None, :], (128, 2))),
        "dtb": np.ascontiguousarray(np.broadcast_to(dt_bias[gh][None, :], (128, 2))),
        "ggdn": np.ascontiguousarray(gdn_norm_g.reshape(128, 1)),
        "cst": consts(),
    }


D = 1024
KD = 8
EPS = 1e-6
NCOL = 1800
TMC = 264


def build_A(TLEN):
    nc = bass.Bass("TRN2", target_bir_lowering=False)
    P = Prog(nc)
    NT = TLEN // 512
    NKT = TLEN // 128
    dt = nc.dram_tensor
    xT_d = dt("xT", [D, TLEN], F32, kind="ExternalInput").ap().rearrange("(k p) n -> p k n", p=128)
    WA_d = dt("WA", [D, NCOL], F32, kind="ExternalInput").ap().rearrange("(k p) n -> p k n", p=128)
    g1_d = dt("g1", [128, KD], F32, kind="ExternalInput").ap()
    fb_d = dt("fbias", [128, 4], F32, kind="ExternalInput").ap()
    gfox_d = dt("gfox", [64, 4], F32, kind="ExternalInput").ap()
    cw_d = dt("convw", [128, 24], F32, kind="ExternalInput").ap()
    alog_d = dt("alog", [128, 2], F32, kind="ExternalInput").ap()
    dtb_d = dt("dtb", [128, 2], F32, kind="ExternalInput").ap()
    ggdn_d = dt("ggdn", [128, 1], F32, kind="ExternalInput").ap()
    cst_d = dt("cst", [128, 5, 128], F32, kind="ExternalInput").ap()
    mixT_d = dt("mixT", [512, TLEN], F32, kind="ExternalOutput").ap()

    sb = P.sb
    WAb = sb("WAb", [128, KD, NCOL], BF16)
    xt = sb("xt", [128, KD, 512], F32)
    hT = sb("hT", [128, KD, 512], BF16)
    KT = sb("KT", [128, 2, TLEN], BF16)
    VA = sb("VA", [128, NKT, 4, 65], BF16)
    QA = sb("QA", [128, 2, 512], BF16)
    QB = sb("QB", [128, 2, 512], BF16)
    cK = sb("cK", [128, NKT, 4], F32)
    biasJ = sb("biasJ", [128, 4, NKT], F32)
    carry = sb("carry", [128, 4], F32)
    crefb = sb("crefb", [128, 4], F32)
    SM = sb("SM", [128, 4, 8], F32)
    GB = sb("GB", [128, 4, 6], F32)
    tf = sb("tf", [128, 8], F32)
    CV = sb("CV", [128, 6, 515], F32)
    cvt = [sb("cvt%d" % i, [128, 512], F32) for i in range(1)]
    cvs = [sb("cvs%d" % i, [128, 512], F32) for i in range(1)]
    qkvn = sb("qkvn", [128, 6, 512], BF16)
    gate = sb("gate", [128, 2, 512], BF16)
    PT = [sb("PT%d" % i, [128, 512], BF16) for i in range(3)]
    sqb = [sb("sqb%d" % i, [128, 512], BF16) for i in range(2)]
    sqf = sb("sqf", [128, 512], F32)
    fo_st = [sb("fo_st%d" % i, [128, 512], F32) for i in range(2)]
    OT = [sb("OT%d" % i, [128, 512], F32) for i in range(2)]
    g1s = sb("g1s", [128, KD], F32)
    fbs = sb("fbs", [128, 4], F32)
    gfoxs = sb("gfoxs", [64, 4], F32)
    cws = sb("cws", [128, 24], F32)
    nA = sb("nA", [128, 2], F32)
    dtbs = sb("dtbs", [128, 2], F32)
    ggdns = sb("ggdns", [128, 1], F32)
    cst = sb("cst_s", [128, 5, 128], F32)
    cstb = sb("cstb", [128, 5, 128], BF16)
    ones_b = sb("ones_b", [128, 128], BF16)
    ones_f = sb("ones_f", [128, 128], F32)
    epsc = sb("epsc", [128, 1], F32)
    onec = sb("onec", [128, 1], F32)
    sel65 = sb("sel65", [128, 64], F32)
    w64b = sb("w64b", [128, 64], BF16)
    hmask = sb("hmask", [128, 2], F32)
    S32 = [sb("S32_%d" % i, [128, 128], F32) for i in range(2)]
    Sb = [sb("Sb_%d" % i, [128, 128], BF16) for i in range(2)]
    rstd = sb("rstd", [128, 512], F32)
    rn = rstd
    NI = 2
    gi = []
    for i in range(NI):
        d = {}
        for nm in ["X", "XT", "Xn", "XTn", "Pm", "Pn", "IT", "kgc", "kg0", "kg1", "vtok", "qgT", "nwT", "vnew"]:
            d[nm] = sb("gi%d_%s" % (i, nm), [128, 128], BF16)
        for nm in ["R", "LT", "EB", "LTs", "LTi"]:
            d[nm] = sb("gi%d_%s" % (i, nm), [128, 128], F32)
        d["sc"] = sb("gi%d_sc" % i, [128, 8], F32)
        gi.append(d)

    ident_f, tri_f, triblk_f, strict_f, onesblk_f = [cst[:, i, :] for i in range(5)]
    ident_b, tri_b = cstb[:, 0, :], cstb[:, 1, :]

    pp = [P.ps("pp%d" % i, [128, 512], F32) for i in range(2)]
    pst = [P.ps("pst%d" % i, [128, 512], F32) for i in range(2)]
    pot = [P.ps("pot%d" % i, [128, 512], F32) for i in range(2)]
    pg_t = P.ps("pg", [128, 512], F32)
    pgb_t = P.ps("pgb", [128, 1024], BF16)

    class Rg:
        def __init__(self, ap, name, dep=None):
            self.ap = ap
            self.d = dep if dep is not None else Dep(name)

        def __getitem__(self, idx):
            return self.ap[idx]

    pg = [Rg(t_[:, 0:128], "pgv", t_.d) for t_ in (pg_t, pst[0], pst[1])]
    pg = pg + [pg[0]]
    pgb = [Rg(pgb_t[:, i * 128:(i + 1) * 128], "pgb%d" % i, pgb_t.d) for i in range(8)]
    pov = [Rg(pot[i][:, 0:128], "pov", pot[i].d) for i in range(2)]
    P._deps_orig = P._deps

    def _deps(lst):
        out = []
        for x in lst:
            if x is None:
                continue
            if isinstance(x, (T, Rg)):
                out.append(x.d)
            else:
                out.append(x)
        return out
    P._deps = _deps

    cnt = {}

    def nxt(key, n=2):
        v = cnt.get(key, 0)
        cnt[key] = v + 1
        return v % n

    def mm(out, lhsT, rhs, start, stop, reads, writes):
        P.op("pe", lambda e: e.matmul(out, lhsT=lhsT, rhs=rhs, start=start, stop=stop), reads=reads, writes=writes)

    def tr(out, in_, idn, reads, writes):
        P.op("pe", lambda e: e.transpose(out, in_, idn), reads=reads, writes=writes)

    def act(out, in_, func, reads, writes, bias=None, scale=1.0):
        if bias is None:
            P.op("act", lambda e: e.activation(out, in_, func, scale=scale), reads=reads, writes=writes)
        else:
            P.op("act", lambda e: e.activation(out, in_, func, bias=bias, scale=scale), reads=reads, writes=writes)

    def ts(out, in0, s1, s2, op0, op1, reads, writes, eng="dve"):
        if op1 is None:
            P.op(eng, lambda e: e.tensor_scalar(out, in0, s1, None, op0), reads=reads, writes=writes)
        else:
            P.op(eng, lambda e: e.tensor_scalar(out, in0, s1, s2, op0, op1), reads=reads, writes=writes)

    def tt(out, in0, in1, op, reads, writes, eng="dve"):
        P.op(eng, lambda e: e.tensor_tensor(out, in0, in1, op), reads=reads, writes=writes)

    def stt(out, in0, sc, in1, op0, op1, reads, writes):
        P.op("dve", lambda e: e.scalar_tensor_tensor(out, in0, sc, in1, op0, op1), reads=reads, writes=writes)

    def cp(out, in_, reads, writes, eng="dve"):
        if eng == "act":
            P.op("act", lambda e: e.copy(out, in_), reads=reads, writes=writes)
        else:
            P.op(eng, lambda e: e.tensor_copy(out, in_), reads=reads, writes=writes)

    def ms(t, ap, val, eng="dve"):
        P.op(eng, lambda e: e.memset(ap, val), writes=[t])

    for t_, d_ in [(g1s, g1_d), (fbs, fb_d), (gfoxs, gfox_d), (cws, cw_d), (nA, alog_d), (dtbs, dtb_d), (ggdns, ggdn_d)]:
        P.dma("sp", lambda e, t_=t_, d_=d_: e.dma_start(out=t_[:], in_=d_), writes=[t_])
    P.dma("sp", lambda e: e.dma_start(out=cst[:], in_=cst_d), writes=[cst])
    P.dma("pool", lambda e: e.dma_start(out=cstb[:], in_=cst_d), writes=[cstb])
    for k in range(KD):
        P.dma("pool", lambda e, k=k: e.dma_start(out=WAb[:, k, :], in_=WA_d[:, k, :]), writes=[WAb])
    ms(ones_b, ones_b[:], 1.0)
    ms(ones_f, ones_f[:], 1.0)
    ms(epsc, epsc[:], EPS)
    ms(onec, onec[:], 1.0)
    ms(sel65, sel65[:], 0.0)
    ms(sel65, sel65[64:65, :], 1.0)
    ms(w64b, w64b[:], 1.0)
    ms(hmask, hmask[:], 0.0)
    ms(hmask, hmask[0:64, 0:1], 1.0)
    ms(hmask, hmask[64:128, 1:2], 1.0)
    ms(QA, QA[:], 0.0, "pool")
    ms(QB, QB[:], 0.0, "pool")
    ms(VA, VA[:], 1.0, "pool")
    ms(CV, CV[:], 0.0, "pool")
    ms(carry, carry[:], 0.0)
    for i in range(2):
        ms(S32[i], S32[i][:], 0.0)
        ms(Sb[i], Sb[i][:], 0.0)
    for i in range(NI):
        ms(gi[i]["vnew"], gi[i]["vnew"][:], 0.0, "pool")
    act(nA[:], nA[:], AF.Exp, [nA], [nA])
    ts(nA[:], nA[:], -1.0, None, ALU.mult, None, [nA], [nA])

    for j in range(NT):
        t0 = j * 512
        P.dma("sp", lambda e, t0=t0: e.dma_start(out=xt[:], in_=xT_d[:, :, t0:t0 + 512]), writes=[xt])
        pmt = pp[nxt("pp")]
        for k in range(KD):
            s = sqb[nxt("sqb")]
            act(s[:], xt[:, k, :], AF.Square, [xt], [s])
            mm(pmt[:], ones_b[:], s[:], k == 0, k == KD - 1, [s, ones_b], [pmt])
        act(rstd[:], pmt[:], AF.Sqrt, [pmt, epsc], [rstd], bias=epsc[:], scale=1.0 / D)
        P.op("dve", lambda e: e.reciprocal(rstd[:], rstd[:]), reads=[rstd], writes=[rstd])
        for k in range(KD):
            stt(hT[:, k, :], xt[:, k, :], g1s[:, k:k + 1], rstd[:], ALU.mult, ALU.mult, [xt, g1s, rstd], [hT])
        for grp in range(12):
            p = pp[nxt("pp")]
            for k in range(KD):
                mm(p[:], WAb[:, k, grp * 128:(grp + 1) * 128], hT[:, k, :], k == 0, k == KD - 1, [WAb, hT], [p])
            if grp < 2:
                cp(QA[0:64, grp, :], p[0:64, :], [p], [QA], "act")
                cp(QB[64:128, grp, :], p[64:128, :], [p], [QB], "act")
            elif grp < 4:
                cp(KT[:, grp - 2, t0:t0 + 512], p[:], [p], [KT], "act")
            elif grp < 10:
                cp(CV[:, grp - 4, 3:515], p[:], [p], [CV], "dve")
            else:
                act(gate[:, grp - 10, :], p[:], AF.Silu, [p], [gate])
        for s_ in range(4):
            p = pp[nxt("pp")]
            for k in range(KD):
                mm(p[:, 0:TMC], hT[:, k, s_ * 128:(s_ + 1) * 128], WAb[:, k, 1536:1536 + TMC], k == 0, k == KD - 1, [WAb, hT], [p])
            kt = 4 * j + s_
            for h4 in range(4):
                cp(VA[:, kt, h4, 0:64], p[:, h4 * 64:(h4 + 1) * 64], [p], [VA], "dve")
            cp(SM[:, s_, :], p[:, 256:264], [p], [SM], "dve")
            tt(tf[:, 0:4], SM[:, s_, 0:4], fbs[:], ALU.add, [SM, fbs], [tf])
            act(tf[:, 0:4], tf[:, 0:4], AF.Exp, [tf], [tf], scale=-1.0)
            act(tf[:, 0:4], tf[:, 0:4], AF.Ln, [tf, onec], [tf], bias=onec[:])
            pq = pg[nxt("pg", 3)]
            mm(pq[:, 0:4], tri_f, tf[:, 0:4], True, True, [cst, tf], [pq])
            mm(pq[:, 4:8], ones_f[:], tf[:, 0:4], True, True, [ones_f, tf], [pq])
            tt(cK[:, kt, :], pq[:, 0:4], carry[:], ALU.add, [pq, carry], [cK])
            tt(carry[:], pq[:, 4:8], carry[:], ALU.add, [pq, carry], [carry])
            if s_ == 1:
                cp(crefb[:], carry[:], [carry], [crefb])
            tt(tf[:, 4:6], SM[:, s_, 4:6], dtbs[:], ALU.add, [SM, dtbs], [tf])
            act(tf[:, 4:6], tf[:, 4:6], AF.Exp, [tf], [tf])
            act(tf[:, 4:6], tf[:, 4:6], AF.Ln, [tf, onec], [tf], bias=onec[:])
            tt(GB[:, s_, 0:2], tf[:, 4:6], nA[:], ALU.mult, [tf, nA], [GB])
            act(GB[:, s_, 2:4], SM[:, s_, 6:8], AF.Sigmoid, [SM], [GB])
            ts(GB[:, s_, 4:6], GB[:, s_, 2:4], -1.0, None, ALU.mult, None, [GB], [GB])

        nkt = 4 * j + 4
        for h in range(4):
            pr, hp = h // 2, h % 2
            Qs = QA if hp == 0 else QB
            ts(biasJ[:, h, 0:nkt], cK[:, 0:nkt, h], crefb[:, h:h + 1], None, ALU.subtract, None, [cK, crefb], [biasJ])
            po = pot[nxt("pot")]
            for kt in range(nkt):
                dg_ = kt - 4 * j
                qlo = 128 * dg_ if dg_ > 0 else 0
                ps_ = pst[nxt("pst")]
                mm(ps_[:, qlo:512], KT[:, pr, kt * 128:(kt + 1) * 128], Qs[:, pr, qlo:512], True, True, [KT, Qs], [ps_])
                pt = PT[nxt("PT", 3)]
                act(pt[:, qlo:512], ps_[:, qlo:512], AF.Exp, [ps_, biasJ], [pt], bias=biasJ[:, h, kt:kt + 1], scale=0.125)
                if dg_ >= 0:
                    tt(pt[:, qlo:qlo + 128], pt[:, qlo:qlo + 128], tri_b, ALU.mult, [pt, cstb], [pt], eng="pool")
                mm(po[0:65, qlo:512], VA[:, kt, h, :], pt[:, qlo:512], kt == 0, kt == nkt - 1, [VA, pt], [po])
            cp(sqf[0:65, :], po[0:65, :], [po], [sqf], "act")
            pn = pp[nxt("pp")]
            mm(pn[0:64, :], sel65[0:65, :], sqf[0:65, :], True, True, [sel65, sqf], [pn])
            P.op("dve", lambda e, pn=pn: e.reciprocal(rn[0:64, :], pn[0:64, :]), reads=[pn], writes=[rn])
            tt(sqf[0:64, :], sqf[0:64, :], rn[0:64, :], ALU.mult, [sqf, rn], [sqf])
            q_ = sqb[nxt("sqb")]
            act(q_[0:64, :], sqf[0:64, :], AF.Square, [sqf], [q_])
            pn2 = pp[nxt("pp")]
            mm(pn2[0:64, :], w64b[0:64, :], q_[0:64, :], True, True, [w64b, q_], [pn2])
            act(rn[0:64, :], pn2[0:64, :], AF.Sqrt, [pn2, epsc], [rn], bias=epsc[0:64, :], scale=1.0 / 64)
            P.op("dve", lambda e: e.reciprocal(rn[0:64, :], rn[0:64, :]), reads=[rn], writes=[rn])
            fs = fo_st[nxt("fo")]
            stt(fs[0:64, :], sqf[0:64, :], gfoxs[:, h:h + 1], rn[0:64, :], ALU.mult, ALU.mult, [sqf, gfoxs, rn], [fs])
            P.dma("sp", lambda e, fs=fs, h=h, t0=t0: e.dma_start(out=mixT_d[h * 64:(h + 1) * 64, t0:t0 + 512], in_=fs[0:64, :]), reads=[fs])

        for i in range(6):
            c_ = cvt[0]
            ts(c_[:], CV[:, i, 0:512], cws[:, i * 4:i * 4 + 1], None, ALU.mult, None, [CV, cws], [c_])
            for k in range(1, 4):
                stt(c_[:], CV[:, i, k:k + 512], cws[:, i * 4 + k:i * 4 + k + 1], c_[:], ALU.mult, ALU.add, [CV, cws, c_], [c_])
            if i % 3 == 2:
                act(qkvn[:, i, :], c_[:], AF.Silu, [c_], [qkvn])
            else:
                s_ = cvs[0]
                act(s_[:], c_[:], AF.Silu, [c_], [s_])
                q_ = sqb[nxt("sqb")]
                act(q_[:], s_[:], AF.Square, [s_], [q_])
                p = pp[nxt("pp")]
                mm(p[:], ones_b[:], q_[:], True, True, [ones_b, q_], [p])
                act(rstd[:], p[:], AF.Sqrt, [p, epsc], [rstd], bias=epsc[:])
                P.op("dve", lambda e: e.reciprocal(rstd[:], rstd[:]), reads=[rstd], writes=[rstd])
                stt(qkvn[:, i, :], s_[:], (128 ** -0.5) if i % 3 == 0 else 1.0, rstd[:], ALU.mult, ALU.mult, [s_, rstd], [qkvn])
        cp(CV[:, :, 0:3], CV[:, :, 512:515], [CV], [CV])

        for s_ in range(4):
            c0 = s_ * 128
            cs_ = slice(c0, c0 + 128)
            for hd in range(2):
                G = gi[hd]
                qT, kT, vT = qkvn[:, 3 * hd, cs_], qkvn[:, 3 * hd + 1, cs_], qkvn[:, 3 * hd + 2, cs_]
                g_ = GB[:, s_, hd:hd + 1]
                beta = GB[:, s_, 2 + hd:3 + hd]
                nbeta = GB[:, s_, 4 + hd:5 + hd]
                sc = G["sc"]
                pq = pg[nxt("pg", 3)]
                mm(pq[:, 0:1], triblk_f, g_, True, True, [cst, GB], [pq])
                mm(pq[:, 1:2], onesblk_f, g_, True, True, [cst, GB], [pq])
                cp(sc[:, 0:2], pq[:, 0:2], [pq], [sc])
                ts(G["R"][:], triblk_f, g_, None, ALU.mult, None, [cst, GB], [G["R"]])
                pq = pg[nxt("pg", 3)]
                mm(pq[:], ones_f[:], G["R"][:], True, True, [ones_f, G["R"]], [pq])
                ts(G["LT"][:], pq[:], sc[:, 0:1], 0.0, ALU.subtract, ALU.min, [pq, sc], [G["LT"]])
                act(G["LT"][:], G["LT"][:], AF.Exp, [G["LT"]], [G["LT"]])
                act(G["EB"][:], pq[:], AF.Exp, [pq], [G["EB"]])
                tt(G["LTs"][:], G["LT"][:], strict_f, ALU.mult, [G["LT"], cst], [G["LTs"]], eng="pool")
                tt(G["LTi"][:], G["LT"][:], triblk_f, ALU.mult, [G["LT"], cst], [G["LTi"]], eng="pool")
                act(sc[:, 2:3], sc[:, 0:1], AF.Exp, [sc], [sc])
                act(sc[:, 3:4], sc[:, 0:1], AF.Exp, [sc], [sc], bias=sc[:, 1:2], scale=-1.0)
                ts(sc[:, 4:6], hmask[:], sc[:, 3:4], None, ALU.mult, None, [hmask, sc], [sc])
                pk = pgb[nxt("pgb", 8)]
                tr(pk[:], kT, ident_b, [qkvn, cstb], [pk])
                ts(G["kgc"][:], pk[:], sc[:, 2:3], None, ALU.mult, None, [pk, sc], [G["kgc"]])
                ts(G["kg0"][:], pk[:], sc[:, 4:5], None, ALU.mult, None, [pk, sc], [G["kg0"]])
                ts(G["kg1"][:], pk[:], sc[:, 5:6], None, ALU.mult, None, [pk, sc], [G["kg1"]])
                pv = pgb[nxt("pgb", 8)]
                tr(pv[:], vT, ident_b, [qkvn, cstb], [pv])
                cp(G["vtok"][:], pv[:], [pv], [G["vtok"]], "act")
                pq = pg[nxt("pg", 3)]
                mm(pq[:], kT, kT, True, True, [qkvn], [pq])
                stt(G["X"][:], pq[:], nbeta, G["LTs"][:], ALU.mult, ALU.mult, [pq, GB, G["LTs"]], [G["X"]])
                pq = pg[nxt("pg", 3)]
                mm(pq[:], kT, qT, True, True, [qkvn], [pq])
                tt(G["IT"][:], pq[:], G["LTi"][:], ALU.mult, [pq, G["LTi"]], [G["IT"]])
                tt(G["qgT"][:], qT, G["EB"][:], ALU.mult, [qkvn, G["EB"]], [G["qgT"]])
                pk = pgb[nxt("pgb", 8)]
                tr(pk[:], G["X"][:], ident_b, [G["X"], cstb], [pk])
                cp(G["XT"][:], pk[:], [pk], [G["XT"]], "act")
                tt(G["Pm"][:], G["X"][:], ident_b, ALU.add, [G["X"], cstb], [G["Pm"]])
            X = ["X", "X"]
            XTn_ = ["XT", "XT"]
            Pn_ = ["Pm", "Pm"]
            for lvl in range(5):
                for hd in range(2):
                    G = gi[hd]
                    xa, xta = G[X[hd]], G[XTn_[hd]]
                    xb_n, xtb_n = ("Xn", "XTn") if X[hd] == "X" else ("X", "XT")
                    xb, xtb = G[xb_n], G[xtb_n]
                    pa_n = Pn_[hd]
                    pb_n = "Pn" if pa_n == "Pm" else "Pm"
                    pq = pg[nxt("pg", 3)]
                    mm(pq[:], xta[:], xa[:], True, True, [xta, xa], [pq])
                    cp(xb[:], pq[:], [pq], [xb], "act")
                    pq2 = pg[nxt("pg", 3)]
                    mm(pq2[:], xa[:], xta[:], True, True, [xta, xa], [pq2])
                    cp(xtb[:], pq2[:], [pq2], [xtb], "dve")
                    pq3 = pg[nxt("pg", 3)]
                    mm(pq3[:], xtb[:], G[pa_n][:], True, True, [xtb, G[pa_n]], [pq3])
                    tt(G[pb_n][:], pq3[:], G[pa_n][:], ALU.add, [pq3, G[pa_n]], [G[pb_n]])
                    X[hd], XTn_[hd], Pn_[hd] = xb_n, xtb_n, pb_n
            for hd in range(2):
                G = gi[hd]
                Pf = G[Pn_[hd]]
                pq = pg[nxt("pg", 3)]
                mm(pq[:], G["kgc"][:], Pf[:], True, True, [G["kgc"], Pf], [pq])
                ts(G["nwT"][:], pq[:], -1.0, None, ALU.mult, None, [pq], [G["nwT"]])
            po_ = [pov[0], pov[1]]
            for X_ in range(2):
                rs_ = slice(64 * X_, 64 * X_ + 64)
                for hd in range(2):
                    G = gi[hd]
                    Pf = G[Pn_[hd]]
                    beta = GB[:, s_, 2 + hd:3 + hd]
                    pq = pg[nxt("pg", 3)]
                    mm(pq[:], Pf[:], G["vtok"][:], True, False, [Pf, G["vtok"]], [pq])
                    mm(pq[:], G["nwT"][:], Sb[hd][:], False, True, [G["nwT"], Sb[hd]], [pq])
                    ts(G["vnew"][rs_, :], pq[rs_, :], GB[rs_, s_, 2 + hd:3 + hd], None, ALU.mult, None, [pq, GB], [G["vnew"]])
                    pq2 = po_[hd]
                    mm(pq2[:, rs_], Sb[hd][:], G["qgT"][:, rs_], True, False, [Sb[hd], G["qgT"]], [pq2])
                    mm(pq2[:, rs_], G["vnew"][:], G["IT"][:, rs_], False, True, [G["vnew"], G["IT"]], [pq2])
                    pq3 = pg[nxt("pg", 3)]
                    kgx = G["kg0"] if X_ == 0 else G["kg1"]
                    mm(pq3[:], kgx[:], G["vnew"][:], True, True, [kgx, G["vnew"]], [pq3])
                    stt(S32[hd][:], S32[hd][:], G["EB"][:, 64 * X_ + 63:64 * X_ + 64], pq3[:], ALU.mult, ALU.add, [S32[hd], G["EB"], pq3], [S32[hd]])
                    cp(Sb[hd][:], S32[hd][:], [S32[hd]], [Sb[hd]], "act")
            for hd in range(2):
                cp(OT[hd][:, cs_], po_[hd][:], [po_[hd]], [OT[hd]], "act")
        for hd in range(2):
            q_ = sqb[nxt("sqb")]
            act(q_[:], OT[hd][:], AF.Square, [OT[hd]], [q_])
            p = pp[nxt("pp")]
            mm(p[:], ones_b[:], q_[:], True, True, [ones_b, q_], [p])
            act(rstd[:], p[:], AF.Sqrt, [p, epsc], [rstd], bias=epsc[:], scale=1.0 / 128)
            P.op("dve", lambda e: e.reciprocal(rstd[:], rstd[:]), reads=[rstd], writes=[rstd])
            gs = OT[hd]
            stt(gs[:], OT[hd][:], ggdns[:, 0:1], rstd[:], ALU.mult, ALU.mult, [OT[hd], ggdns, rstd], [gs])
            tt(gs[:], gs[:], gate[:, hd, :], ALU.mult, [gs, gate], [gs])
            P.dma("sp", lambda e, gs=gs, hd=hd, t0=t0: e.dma_start(out=mixT_d[256 + hd * 128:256 + (hd + 1) * 128, t0:t0 + 512], in_=gs[:]), reads=[gs])
    evs = []
    for t_ in fo_st + OT:
        evs += list(t_.d.rs)
    P.finish("sp", evs)
    P.emit()
    return nc


D = 1024
KD = 8
EPS = 1e-6


def build_B(NTOK, TG, E, F, moe, final):
    nc = bass.Bass("TRN2", target_bir_lowering=False)
    P = Prog(nc)
    NF = F // 128
    NTT = TG // 512
    NG = NTOK // TG
    dt = nc.dram_tensor
    xT_d = dt("xT", [D, NTOK], F32, kind="ExternalInput").ap().rearrange("(k p) n -> p k n", p=128)
    mixT_d = dt("mixT", [D, NTOK], F32, kind="ExternalInput").ap().rearrange("(k p) n -> p k n", p=128)
    wout_d = dt("wout", [D, D], F32, kind="ExternalInput").ap().rearrange("(k p) n -> p k n", p=128)
    g2_d = dt("g2", [128, KD], F32, kind="ExternalInput").ap()
    gf_d = dt("gf", [128, KD], F32, kind="ExternalInput").ap()
    w1_d = dt("w1", [E, D, F], F32, kind="ExternalInput").ap()
    w3_d = dt("w3", [E, D, F], F32, kind="ExternalInput").ap()
    w2_d = dt("w2", [E, F, D], F32, kind="ExternalInput").ap()
    wr_d = dt("wr", [D, 8], F32, kind="ExternalInput").ap().rearrange("(k p) n -> p k n", p=128)
    id_d = dt("ident", [128, 128], F32, kind="ExternalInput").ap()
    yT_d = dt("yT", [D, NTOK], F32, kind="ExternalOutput").ap().rearrange("(k p) n -> p k n", p=128)
    if final:
        yfT_d = dt("yfT", [D, NTOK], F32, kind="ExternalOutput").ap().rearrange("(k p) n -> p k n", p=128)

    xs = P.sb("xs", [128, KD, TG], F32)
    hT = P.sb("hT", [128, KD, TG], BF16)
    gT = P.sb("gT", [128, NF, TG], BF16)
    mixs = gT
    woutb = P.sb("woutb", [128, KD, D], BF16)
    w1b = [P.sb("w1b%d" % i, [128, KD, 256], BF16) for i in range(2)]
    w3b = [P.sb("w3b%d" % i, [128, KD, 256], BF16) for i in range(2)]
    w2b = [P.sb("w2b%d" % i, [128, NF, 128], BF16) for i in range(2)]
    g2s = P.sb("g2s", [128, KD], F32)
    gfs = P.sb("gfs", [128, KD], F32)
    ident = P.sb("ident_s", [128, 128], F32)
    ones_b = P.sb("ones_b", [128, 128], BF16)
    ones_f = P.sb("ones_f", [128, 128], F32)
    epsc = P.sb("epsc", [128, 1], F32)
    sq = [P.sb("sq%d" % i, [128, 512], BF16) for i in range(2)]
    rstd = P.sb("rstd", [128, 512], F32)
    sil = [P.sb("sil%d" % i, [128, 512], F32) for i in range(2)]
    tmpb = [P.sb("tmpb%d" % i, [128, 512], F32) for i in range(2)]
    if moe:
        h32 = [P.sb("h32_%d" % i, [128, 512], F32) for i in range(2)]
        wrs = P.sb("wrs", [128, KD, 8], F32)
        lg = P.sb("lg", [128, 8], F32)
        lgT = P.sb("lgT", [8, 512], F32)
        top8 = P.sb("top8", [128, 8], F32)
        rw = P.sb("rw", [128, TG // 128, 8], F32)
        tmp8 = P.sb("tmp8", [128, 8], F32)
        tmp1 = P.sb("tmp1", [128, 2], F32)
        dg = [P.sb("dg%d" % i, [128, 128], F32) for i in range(2)]
        wbc = P.sb("wbc", [128, TG], F32)
    pa = [P.ps("pa%d" % i, [128, 512], F32) for i in range(2)]
    pb = [P.ps("pb%d" % i, [128, 512], F32) for i in range(2)]
    py = [P.ps("py%d" % i, [128, 512], F32) for i in range(2)]
    pm = [P.ps("pm%d" % i, [128, 512], F32) for i in range(2)]

    P.dma("sp", lambda e: e.dma_start(out=ident[:], in_=id_d), writes=[ident])
    P.dma("sp", lambda e: e.dma_start(out=g2s[:], in_=g2_d), writes=[g2s])
    P.dma("sp", lambda e: e.dma_start(out=gfs[:], in_=gf_d), writes=[gfs])
    P.op("dve", lambda e: e.memset(ones_b[:], 1.0), writes=[ones_b])
    P.op("dve", lambda e: e.memset(ones_f[:], 1.0), writes=[ones_f])
    P.op("dve", lambda e: e.memset(epsc[:], EPS), writes=[epsc])
    for k in range(KD):
        P.dma("pool", lambda e, k=k: e.dma_start(out=woutb[:, k, :], in_=wout_d[:, k, :]), writes=[woutb])
    if moe:
        P.dma("sp", lambda e: e.dma_start(out=wrs[:], in_=wr_d), writes=[wrs])

    cnt = {"sq": 0, "pm": 0, "pa": 0, "py": 0, "w13": 0, "w2": 0, "sil": 0, "dg": 0, "h32": 0}

    def nxt(key, n=2):
        v = cnt[key] % n
        cnt[key] += 1
        return v

    def rmsnorm(src, gs, dst_bf, tt, dst32=None):
        sl = slice(tt * 512, (tt + 1) * 512)
        pmt = pm[nxt("pm")]
        for k in range(KD):
            s = sq[nxt("sq")]
            P.op("act", lambda e, s=s, k=k: e.activation(s[:], src[:, k, sl], AF.Square), reads=[src], writes=[s])
            P.op("pe", lambda e, s=s, k=k: e.matmul(pmt[:], lhsT=ones_b[:], rhs=s[:], start=(k == 0), stop=(k == KD - 1)),
                 reads=[s, ones_b], writes=[pmt])
        P.op("act", lambda e: e.activation(rstd[:], pmt[:], AF.Sqrt, bias=epsc[:], scale=1.0 / D), reads=[pmt, epsc], writes=[rstd])
        P.op("dve", lambda e: e.reciprocal(rstd[:], rstd[:]), reads=[rstd], writes=[rstd])
        for k in range(KD):
            P.op("dve", lambda e, k=k: e.scalar_tensor_tensor(dst_bf[:, k, sl], src[:, k, sl], gs[:, k:k + 1], rstd[:], ALU.mult, ALU.mult),
                 reads=[src, gs, rstd], writes=[dst_bf])

    for gi in range(NG):
        t0 = gi * TG
        P.dma("sp", lambda e, t0=t0: e.dma_start(out=xs[:], in_=xT_d[:, :, t0:t0 + TG]), writes=[xs])
        for k in range(KD):
            P.dma("pool", lambda e, t0=t0, k=k: e.dma_start(out=mixs[:, k, :], in_=mixT_d[:, k, t0:t0 + TG]), writes=[mixs])
        for g in range(KD):
            for tt in range(NTT):
                sl = slice(tt * 512, (tt + 1) * 512)
                p = py[nxt("py")]
                for k in range(KD):
                    P.op("pe", lambda e, p=p, k=k, g=g, sl=sl: e.matmul(p[:], lhsT=woutb[:, k, g * 128:(g + 1) * 128], rhs=mixs[:, k, sl],
                                                                     start=(k == 0), stop=(k == KD - 1)), reads=[woutb, mixs], writes=[p])
                P.op("dve", lambda e, p=p, g=g, sl=sl: e.tensor_tensor(xs[:, g, sl], xs[:, g, sl], p[:], ALU.add), reads=[xs, p], writes=[xs])
        for tt in range(NTT):
            rmsnorm(xs, g2s, hT, tt)
            if moe:
                sl_ = slice(tt * 512, (tt + 1) * 512)
                prt = pm[nxt("pm")]
                for k in range(KD):
                    hk = h32[nxt("h32")]
                    P.op("dve", lambda e, k=k, hk=hk, sl_=sl_: e.scalar_tensor_tensor(hk[:], xs[:, k, sl_], g2s[:, k:k + 1], rstd[:], ALU.mult, ALU.mult),
                         reads=[xs, g2s, rstd], writes=[hk])
                    P.op("pe", lambda e, k=k, hk=hk, prt=prt: e.matmul(prt[0:8, :], lhsT=wrs[:, k, :], rhs=hk[:], start=(k == 0), stop=(k == KD - 1)),
                         reads=[hk, wrs], writes=[prt])
                P.op("act", lambda e, prt=prt: e.copy(lgT[:], prt[0:8, :]), reads=[prt], writes=[lgT])
                prt = pm[nxt("pm")]
                for ts in range(4):
                    P.op("pe", lambda e, ts=ts, prt=prt: e.transpose(prt[:, ts * 8:(ts + 1) * 8], lgT[0:8, ts * 128:(ts + 1) * 128], ident[0:8, 0:8]),
                         reads=[lgT, ident], writes=[prt])
                for ts in range(4):
                    st = tt * 4 + ts
                    p = prt
                    P.op("dve", lambda e, p=p, ts=ts: e.tensor_copy(lg[:], p[:, ts * 8:(ts + 1) * 8]), reads=[p], writes=[lg])
                    P.op("dve", lambda e: e.max(out=top8[:], in_=lg[:]), reads=[lg], writes=[top8])
                    P.op("dve", lambda e: e.tensor_scalar(tmp1[:, 0:1], top8[:, 0:1], -1.0, None, ALU.mult), reads=[top8], writes=[tmp1])
                    P.op("act", lambda e: e.activation(tmp8[:], lg[:], AF.Exp, bias=tmp1[:, 0:1], scale=1.0), reads=[lg, tmp1], writes=[tmp8])
                    P.op("act", lambda e: e.activation(tmp1[:, 1:2], top8[:, 1:2], AF.Exp, bias=tmp1[:, 0:1], scale=1.0), reads=[top8, tmp1], writes=[tmp1])
                    P.op("dve", lambda e: e.tensor_scalar(tmp1[:, 1:2], tmp1[:, 1:2], 1.0, None, ALU.add), reads=[tmp1], writes=[tmp1])
                    P.op("dve", lambda e: e.reciprocal(tmp1[:, 1:2], tmp1[:, 1:2]), reads=[tmp1], writes=[tmp1])
                    P.op("dve", lambda e: e.tensor_scalar(lg[:], lg[:], top8[:, 1:2], None, ALU.is_ge), reads=[lg, top8], writes=[lg])
                    P.op("dve", lambda e, st=st: e.scalar_tensor_tensor(rw[:, st, :], tmp8[:], tmp1[:, 1:2], lg[:], ALU.mult, ALU.mult),
                         reads=[tmp8, tmp1, lg], writes=[rw])
        tasks = []
        for ex in range(E):
            for fp in range(NF // 2):
                tasks.append(("h", ex, fp))
            for g in range(KD):
                tasks.append(("y", ex, g))
        bufs = {}

        def issue(i):
            kind, ex, j = tasks[i]
            if kind == "h":
                wi = nxt("w13")
                a_, b_ = w1b[wi], w3b[wi]
                bufs[i] = (a_, b_)
                f0 = j * 256
                src1 = w1_d[ex].rearrange("(k p) f -> p k f", p=128)
                src3 = w3_d[ex].rearrange("(k p) f -> p k f", p=128)
                for k in range(KD):
                    P.dma("pool", lambda e, a_=a_, k=k, f0=f0, src1=src1: e.dma_start(out=a_[:, k, :], in_=src1[:, k, f0:f0 + 256]), writes=[a_])
                    P.dma("pool", lambda e, b_=b_, k=k, f0=f0, src3=src3: e.dma_start(out=b_[:, k, :], in_=src3[:, k, f0:f0 + 256]), writes=[b_])
            else:
                w_ = w2b[nxt("w2")]
                bufs[i] = w_
                src2 = w2_d[ex].rearrange("(c p) d -> p c d", p=128)
                P.dma("pool", lambda e, w_=w_, j=j, src2=src2: e.dma_start(out=w_[:], in_=src2[:, :, j * 128:(j + 1) * 128]), writes=[w_])

        issue(0)
        for i, (kind, ex, j) in enumerate(tasks):
            if i + 1 < len(tasks):
                issue(i + 1)
            if kind == "h" and j == 0 and moe:
                for st in range(TG // 128):
                    d_ = dg[nxt("dg")]
                    P.op("dve", lambda e, d_=d_, st=st, ex=ex: e.tensor_scalar(d_[:], ident[:], rw[:, st, ex:ex + 1], None, ALU.mult),
                         reads=[ident, rw], writes=[d_])
                    p = pm[nxt("pm")]
                    P.op("pe", lambda e, p=p, d_=d_: e.matmul(p[:, 0:128], lhsT=ones_f[:], rhs=d_[:], start=True, stop=True), reads=[ones_f, d_], writes=[p])
                    P.op("act", lambda e, p=p, st=st: e.copy(wbc[:, st * 128:(st + 1) * 128], p[:, 0:128]), reads=[p], writes=[wbc])
            if kind == "h":
                a_, b_ = bufs[i]
                for fc in range(2):
                    f = j * 2 + fc
                    for tt in range(NTT):
                        sl = slice(tt * 512, (tt + 1) * 512)
                        pi = nxt("pa")
                        p_a, p_b = pa[pi], pb[pi]
                        for k in range(KD):
                            P.op("pe", lambda e, p_a=p_a, a_=a_, k=k, fc=fc, sl=sl: e.matmul(p_a[:], lhsT=a_[:, k, fc * 128:(fc + 1) * 128], rhs=hT[:, k, sl],
                                                                                      start=(k == 0), stop=(k == KD - 1)), reads=[a_, hT], writes=[p_a])
                        for k in range(KD):
                            P.op("pe", lambda e, p_b=p_b, b_=b_, k=k, fc=fc, sl=sl: e.matmul(p_b[:], lhsT=b_[:, k, fc * 128:(fc + 1) * 128], rhs=hT[:, k, sl],
                                                                                      start=(k == 0), stop=(k == KD - 1)), reads=[b_, hT], writes=[p_b])
                        s_ = sil[nxt("sil")]
                        P.op("act", lambda e, s_=s_, p_a=p_a: e.activation(s_[:], p_a[:], AF.Silu), reads=[p_a], writes=[s_])
                        if moe:
                            t_ = tmpb[pi]
                            P.op("dve", lambda e, t_=t_, p_b=p_b, sl=sl: e.tensor_tensor(t_[:], p_b[:], wbc[:, sl], ALU.mult), reads=[p_b, wbc], writes=[t_])
                            P.op("dve", lambda e, t_=t_, s_=s_, f=f, sl=sl: e.tensor_tensor(gT[:, f, sl], s_[:], t_[:], ALU.mult), reads=[s_, t_], writes=[gT])
                        else:
                            P.op("dve", lambda e, s_=s_, p_b=p_b, f=f, sl=sl: e.tensor_tensor(gT[:, f, sl], s_[:], p_b[:], ALU.mult), reads=[s_, p_b], writes=[gT])
            else:
                w_ = bufs[i]
                g = j
                for tt in range(NTT):
                    sl = slice(tt * 512, (tt + 1) * 512)
                    p = py[nxt("py")]
                    for f in range(NF):
                        P.op("pe", lambda e, p=p, w_=w_, f=f, sl=sl: e.matmul(p[:], lhsT=w_[:, f, :], rhs=gT[:, f, sl], start=(f == 0), stop=(f == NF - 1)),
                             reads=[w_, gT], writes=[p])
                    P.op("dve", lambda e, p=p, g=g, sl=sl: e.tensor_tensor(xs[:, g, sl], xs[:, g, sl], p[:], ALU.add), reads=[xs, p], writes=[xs])
        P.dma("sp", lambda e, t0=t0: e.dma_start(out=yT_d[:, :, t0:t0 + TG], in_=xs[:]), reads=[xs])
        if final:
            for tt in range(NTT):
                rmsnorm(xs, gfs, xs, tt)
            P.dma("sp", lambda e, t0=t0: e.dma_start(out=yfT_d[:, :, t0:t0 + TG], in_=xs[:]), reads=[xs])
    P.finish("sp", list(xs.d.rs))
    P.emit()
    return nc


from concourse.bass_utils import run_bass_kernel_spmd

SEQ = 8192
BATCH = 4
DEPTH = 4
_PROGS = {}


def _prog(key):
    if key not in _PROGS:
        if key == "A":
            _PROGS[key] = build_A(SEQ)
        elif key == "Bd":
            _PROGS[key] = build_B(SEQ // 2, 1024, 1, 2816, False, False)
        else:
            _PROGS[key] = build_B(SEQ // 2, 1024, 8, 3584, True, True)
    return _PROGS[key]


def kernel(x, ln1_g, w_in, fox_f_bias, fox_norm_g, gdn_conv_w, gdn_a_log, gdn_dt_bias,
           gdn_norm_g, w_out, ln2_g, ffn_w1, ffn_w3, ffn_w2, router_w, exp_w1, exp_w3,
           exp_w2, final_g):
    f32 = np.float32
    x = np.asarray(x, f32)
    xT = [np.ascontiguousarray(x[b].T) for b in range(BATCH)]
    ident = np.eye(128, dtype=f32)
    gf = lay8(np.asarray(final_g, f32))
    cores = list(range(8))
    H = SEQ // 2
    outT = None
    for layer in range(DEPTH):
        maps = []
        for b in range(BATCH):
            for hh in range(2):
                maps.append(prep_A(xT[b], np.asarray(ln1_g[layer], f32), np.asarray(w_in[layer], f32),
                                   np.asarray(fox_f_bias[layer], f32), np.asarray(fox_norm_g[layer], f32),
                                   np.asarray(gdn_conv_w[layer], f32), np.asarray(gdn_a_log[layer], f32),
                                   np.asarray(gdn_dt_bias[layer], f32), np.asarray(gdn_norm_g[layer], f32), hh))
        resA = run_bass_kernel_spmd(_prog("A"), maps, core_ids=cores).results
        mixT = []
        for b in range(BATCH):
            m0, m1 = np.asarray(resA[2 * b]["mixT"]), np.asarray(resA[2 * b + 1]["mixT"])
            mixT.append(np.concatenate([m0[0:256], m1[0:256], m0[256:512], m1[256:512]], axis=0))
        j = layer // 2
        moe = (layer % 2 == 1)
        maps = []
        for b in range(BATCH):
            for th in range(2):
                sl = slice(th * H, (th + 1) * H)
                m = {"xT": np.ascontiguousarray(xT[b][:, sl]), "mixT": np.ascontiguousarray(mixT[b][:, sl]),
                     "wout": np.asarray(w_out[layer], f32), "g2": lay8(np.asarray(ln2_g[layer], f32)), "gf": gf, "ident": ident}
                if moe:
                    m.update({"w1": np.asarray(exp_w1[j], f32), "w3": np.asarray(exp_w3[j], f32), "w2": np.asarray(exp_w2[j], f32),
                              "wr": np.asarray(router_w[j], f32)})
                else:
                    m.update({"w1": np.asarray(ffn_w1[j], f32)[None], "w3": np.asarray(ffn_w3[j], f32)[None],
                              "w2": np.asarray(ffn_w2[j], f32)[None], "wr": np.zeros((1024, 8), f32)})
                maps.append(m)
        resB = run_bass_kernel_spmd(_prog("Bm" if moe else "Bd"), maps, core_ids=cores).results
        for b in range(BATCH):
            xT[b] = np.concatenate([np.asarray(resB[2 * b]["yT"]), np.asarray(resB[2 * b + 1]["yT"])], axis=1)
        if layer == DEPTH - 1:
            outT = [np.concatenate([np.asarray(resB[2 * b]["yfT"]), np.asarray(resB[2 * b + 1]["yfT"])], axis=1) for b in range(BATCH)]
    out = np.stack([np.ascontiguousarray(o.T) for o in outT], axis=0).astype(f32)
    return out
```

```python
from contextlib import ExitStack
import numpy as np
import concourse.bass as bass
import concourse.mybir as mybir

F32 = mybir.dt.float32
BF16 = mybir.dt.bfloat16
AF = mybir.ActivationFunctionType
ALU = mybir.AluOpType
AX = mybir.AxisListType

SAME_ENGINE_SYNC = True


class Dep:
    __slots__ = ("w", "rs", "wneeds", "dsem", "name")

    def __init__(self, name=""):
        self.w = None
        self.rs = []
        self.wneeds = []
        self.dsem = None
        self.name = name


class T:
    def __init__(self, h, name, nsub=1):
        self.h = h
        self.name = name
        self.d = Dep(name)
        self.subs = [Dep(name + str(i)) for i in range(nsub)] if nsub > 1 else None

    def __getitem__(self, idx):
        return self.h[idx]


class Prog:
    ENGS = ["pe", "act", "dve", "pool", "sp"]

    def __init__(self, nc):
        self.nc = nc
        self.items = {e: [] for e in self.ENGS}
        self.cnt = {e: 0 for e in self.ENGS}
        self.known = {e: {} for e in self.ENGS}
        self.dsems = []
        self.dcnt = {}
        self.stack = ExitStack()
        self.final_events = []

    def sb(self, name, shape, dtype, nsub=1):
        h = self.stack.enter_context(self.nc.sbuf_tensor(name, list(shape), dtype))
        return T(h, name, nsub)

    def ps(self, name, shape, dtype=F32, nsub=1):
        h = self.stack.enter_context(self.nc.psum_tensor(name, list(shape), dtype))
        return T(h, name, nsub)

    def _need(self, eng, ev, waits):
        if ev is None:
            return
        key, val = ev
        if key == eng and (eng == "pe" or not SAME_ENGINE_SYNC):
            return
        if self.known[eng].get(key, 0) >= val:
            return
        if waits.get(key, 0) < val:
            waits[key] = val

    def _collect(self, eng, reads, writes):
        waits = {}
        for d in reads:
            self._need(eng, d.w, waits)
        wn = {}
        for d in writes:
            evs = [d.w] + list(d.rs)
            for ev in evs:
                self._need(eng, ev, waits)
        for k, v in waits.items():
            self.known[eng][k] = v
        return waits

    def _commit(self, ev, reads, writes):
        for d in reads:
            d.rs.append(ev)
        for d in writes:
            d.w = ev
            d.rs = []

    @staticmethod
    def _deps(lst):
        out = []
        for x in lst:
            if x is None:
                continue
            if isinstance(x, T):
                out.append(x.d)
            else:
                out.append(x)
        return out

    def op(self, eng, fn, reads=(), writes=()):
        reads = self._deps(reads)
        writes = self._deps(writes)
        waits = self._collect(eng, reads, writes)
        self.cnt[eng] += 1
        ev = (eng, self.cnt[eng])
        self._commit(ev, reads, writes)
        self.items[eng].append((fn, waits, eng, 1))

    def dma(self, q, fn, reads=(), writes=(), semdep=None):
        reads = self._deps(reads)
        writes = self._deps(writes)
        if semdep is None:
            semdep = writes[0] if writes else reads[0]
        elif isinstance(semdep, T):
            semdep = semdep.d
        if semdep.dsem is None:
            semdep.dsem = ("dma", len(self.dsems))
            self.dsems.append(semdep)
            self.dcnt[semdep.dsem] = 0
        key = semdep.dsem
        waits = {}
        for d in reads:
            self._need(q, d.w, waits)
        for d in writes:
            evs = list(d.rs)
            if d.w is not None and not (d.w[0] == key and not d.rs):
                evs.append(d.w)
            for ev in evs + (d.wneeds if (d.w is not None and d.w[0] == key and not d.rs) else []):
                self._need(q, ev, waits)
            if not (d.w is not None and d.w[0] == key and not d.rs):
                d.wneeds = evs
        for k, v in waits.items():
            self.known[q][k] = v
        self.dcnt[key] += 16
        ev = (key, self.dcnt[key])
        self._commit(ev, reads, writes)
        self.items[q].append((fn, waits, key, 16))
        return ev

    def finish(self, eng, events):
        waits = {}
        for ev in events:
            self._need(eng, ev, waits)
        self.items[eng].append((None, waits, None, 0))

    def emit(self):
        nc = self.nc
        with ExitStack() as st:
            sems = {}
            for e in self.ENGS:
                sems[e] = st.enter_context(nc.semaphore("s_" + e))
            for i, d in enumerate(self.dsems):
                sems[d.dsem] = st.enter_context(nc.semaphore("d%d" % i))
            block = st.enter_context(nc.Block())

            def replay(ename):
                def f(eng):
                    for fn, waits, inckey, incv in self.items[ename]:
                        for k, v in waits.items():
                            eng.wait_ge(sems[k], v)
                        if fn is not None:
                            ins = fn(eng)
                            ins.then_inc(sems[inckey], incv)
                return f

            block.tensor(replay("pe"))
            block.scalar(replay("act"))
            block.vector(replay("dve"))
            block.gpsimd(replay("pool"))
            block.sync(replay("sp"))
        self.stack.close()


FOX_W = 512
def wa_cols(hh):
    fq, fk, fv, ff = 0, 512, 1024, 1536
    gq, gk, gv = 1544, 1544 + 512, 1544 + 1024
    ga, gb, gz = 3080, 3084, 3088
    heads = [4 * hh + i for i in range(4)]
    gh = [2 * hh, 2 * hh + 1]
    idx = []
    for base in (fq, fk):
        for h in heads:
            idx += list(range(base + h * 64, base + (h + 1) * 64))
    for g in gh:
        for base in (gq, gk, gv):
            idx += list(range(base + g * 128, base + (g + 1) * 128))
    for g in gh:
        idx += list(range(gz + g * 128, gz + (g + 1) * 128))
    for h in heads:
        idx += list(range(fv + h * 64, fv + (h + 1) * 64))
    idx += [ff + h for h in heads]
    idx += [ga + g for g in gh]
    idx += [gb + g for g in gh]
    assert len(idx) == 1800
    return np.array(idx)

def consts():
    i = np.arange(128)
    ident = np.eye(128, dtype=np.float32)
    tri = (i[:, None] <= i[None, :]).astype(np.float32)
    same = (i[:, None] // 64 == i[None, :] // 64)
    triblk = (tri > 0) & same
    strict = (i[:, None] < i[None, :]) & same
    c = np.stack([ident, tri, triblk.astype(np.float32), strict.astype(np.float32), same.astype(np.float32)], axis=1)
    return np.ascontiguousarray(c.astype(np.float32))

def lay8(g):
    return np.ascontiguousarray(g.reshape(8, 128).T)

def prep_A(xTb, ln1_g, w_in, fox_f_bias, fox_norm_g, conv_w, a_log, dt_bias, gdn_norm_g, hh):
    heads = [4 * hh + i for i in range(4)]
    gh = [2 * hh, 2 * hh + 1]
    cw = np.zeros((128, 24), np.float32)
    for g_i, g in enumerate(gh):
        for m in range(3):
            ch0 = m * 512 + g * 128
            cw[:, (g_i * 3 + m) * 4:(g_i * 3 + m) * 4 + 4] = conv_w[:, ch0:ch0 + 128].T
    return {
        "xT": xTb,
        "WA": np.ascontiguousarray(w_in[:, wa_cols(hh)]),
        "g1": lay8(ln1_g),
        "fbias": np.ascontiguousarray(np.broadcast_to(fox_f_bias[heads][None, :], (128, 4))),
        "gfox": np.ascontiguousarray(fox_norm_g.reshape(8, 64)[heads].T),
        "convw": cw,
        "alog": np.ascontiguousarray(np.broadcast_to(a_log[gh][None, :], (128, 2))),
        "dtb": np.ascontiguousarray(np.broadcast_to(dt_bias[gh][None, :], (128, 2))),
        "ggdn": np.ascontiguousarray(gdn_norm_g.reshape(128, 1)),
        "cst": consts(),
    }


D = 1024
KD = 8
EPS = 1e-6
NCOL = 1800
TMC = 264


def build_A(TLEN):
    nc = bass.Bass("TRN2", target_bir_lowering=False)
    P = Prog(nc)
    NT = TLEN // 512
    NKT = TLEN // 128
    dt = nc.dram_tensor
    xT_d = dt("xT", [D, TLEN], F32, kind="ExternalInput").ap().rearrange("(k p) n -> p k n", p=128)
    WA_d = dt("WA", [D, NCOL], F32, kind="ExternalInput").ap().rearrange("(k p) n -> p k n", p=128)
    g1_d = dt("g1", [128, KD], F32, kind="ExternalInput").ap()
    fb_d = dt("fbias", [128, 4], F32, kind="ExternalInput").ap()
    gfox_d = dt("gfox", [64, 4], F32, kind="ExternalInput").ap()
    cw_d = dt("convw", [128, 24], F32, kind="ExternalInput").ap()
    alog_d = dt("alog", [128, 2], F32, kind="ExternalInput").ap()
    dtb_d = dt("dtb", [128, 2], F32, kind="ExternalInput").ap()
    ggdn_d = dt("ggdn", [128, 1], F32, kind="ExternalInput").ap()
    cst_d = dt("cst", [128, 5, 128], F32, kind="ExternalInput").ap()
    mixT_d = dt("mixT", [512, TLEN], F32, kind="ExternalOutput").ap()

    sb = P.sb
    WAb = sb("WAb", [128, KD, NCOL], BF16)
    xt = sb("xt", [128, KD, 512], F32)
    hT = sb("hT", [128, KD, 512], BF16)
    KT = sb("KT", [128, 2, TLEN], BF16)
    VA = sb("VA", [128, NKT, 4, 65], BF16)
    QA = sb("QA", [128, 2, 512], BF16)
    QB = sb("QB", [128, 2, 512], BF16)
    cK = sb("cK", [128, NKT, 4], F32)
    biasJ = sb("biasJ", [128, 4, NKT], F32)
    carry = sb("carry", [128, 4], F32)
    crefb = sb("crefb", [128, 4], F32)
    SM = sb("SM", [128, 4, 8], F32)
    GB = sb("GB", [128, 4, 6], F32)
    tf = sb("tf", [128, 8], F32)
    CV = sb("CV", [128, 6, 515], F32)
    cvt = [sb("cvt%d" % i, [128, 512], F32) for i in range(1)]
    cvs = [sb("cvs%d" % i, [128, 512], F32) for i in range(1)]
    qkvn = sb("qkvn", [128, 6, 512], BF16)
    gate = sb("gate", [128, 2, 512], BF16)
    PT = [sb("PT%d" % i, [128, 512], BF16) for i in range(3)]
    sqb = [sb("sqb%d" % i, [128, 512], BF16) for i in range(2)]
    sqf = sb("sqf", [128, 512], F32)
    fo_st = [sb("fo_st%d" % i, [128, 512], F32) for i in range(2)]
    OT = [sb("OT%d" % i, [128, 512], F32) for i in range(2)]
    g1s = sb("g1s", [128, KD], F32)
    fbs = sb("fbs", [128, 4], F32)
    gfoxs = sb("gfoxs", [64, 4], F32)
    cws = sb("cws", [128, 24], F32)
    nA = sb("nA", [128, 2], F32)
    dtbs = sb("dtbs", [128, 2], F32)
    ggdns = sb("ggdns", [128, 1], F32)
    cst = sb("cst_s", [128, 5, 128], F32)
    cstb = sb("cstb", [128, 5, 128], BF16)
    ones_b = sb("ones_b", [128, 128], BF16)
    ones_f = sb("ones_f", [128, 128], F32)
    epsc = sb("epsc", [128, 1], F32)
    onec = sb("onec", [128, 1], F32)
    sel65 = sb("sel65", [128, 64], F32)
    w64b = sb("w64b", [128, 64], BF16)
    hmask = sb("hmask", [128, 2], F32)
    S32 = [sb("S32_%d" % i, [128, 128], F32) for i in range(2)]
    Sb = [sb("Sb_%d" % i, [128, 128], BF16) for i in range(2)]
    rstd = sb("rstd", [128, 512], F32)
    rn = sb("rn_f", [128, 512], F32)
    sqbf = sb("sqbf", [128, 512], BF16)
    NI = 2
    gi = []
    for i in range(NI):
        d = {}
        for nm in ["X", "XT", "Xn", "XTn", "Pm", "Pn", "IT", "kgc", "kg0", "kg1", "vtok", "qgT", "nwT", "vnew"]:
            d[nm] = sb("gi%d_%s" % (i, nm), [128, 128], BF16)
        for nm in ["R", "LT", "EB", "LTs", "LTi"]:
            d[nm] = sb("gi%d_%s" % (i, nm), [128, 128], F32)
        d["sc"] = sb("gi%d_sc" % i, [128, 8], F32)
        gi.append(d)

    ident_f, tri_f, triblk_f, strict_f, onesblk_f = [cst[:, i, :] for i in range(5)]
    ident_b, tri_b = cstb[:, 0, :], cstb[:, 1, :]

    pp = [P.ps("pp%d" % i, [128, 512], F32) for i in range(2)]
    pst = [P.ps("pst%d" % i, [128, 512], F32) for i in range(2)]
    pot = [P.ps("pot%d" % i, [128, 512], F32) for i in range(2)]
    pg_t = P.ps("pg", [128, 512], F32)
    pgb_t = P.ps("pgb", [128, 1024], BF16)

    class Rg:
        def __init__(self, ap, name, dep=None):
            self.ap = ap
            self.d = dep if dep is not None else Dep(name)

        def __getitem__(self, idx):
            return self.ap[idx]

    pg = [Rg(t_[:, 0:128], "pgv", t_.d) for t_ in (pg_t, pot[1])]
    pgb = [Rg(pgb_t[:, i * 128:(i + 1) * 128], "pgb%d" % i, pgb_t.d) for i in range(8)]
    pov = [Rg(pot[1][:, 128 + 128 * i:256 + 128 * i], "pov", pot[1].d) for i in range(2)]
    P._deps_orig = P._deps

    def _deps(lst):
        out = []
        for x in lst:
            if x is None:
                continue
            if isinstance(x, (T, Rg)):
                out.append(x.d)
            else:
                out.append(x)
        return out
    P._deps = _deps

    cnt = {}

    def nxt(key, n=2):
        v = cnt.get(key, 0)
        cnt[key] = v + 1
        return v % n

    def mm(out, lhsT, rhs, start, stop, reads, writes):
        P.op("pe", lambda e: e.matmul(out, lhsT=lhsT, rhs=rhs, start=start, stop=stop), reads=reads, writes=writes)

    def tr(out, in_, idn, reads, writes):
        P.op("pe", lambda e: e.transpose(out, in_, idn), reads=reads, writes=writes)

    def act(out, in_, func, reads, writes, bias=None, scale=1.0):
        if bias is None:
            P.op("act", lambda e: e.activation(out, in_, func, scale=scale), reads=reads, writes=writes)
        else:
            P.op("act", lambda e: e.activation(out, in_, func, bias=bias, scale=scale), reads=reads, writes=writes)

    def ts(out, in0, s1, s2, op0, op1, reads, writes, eng="dve"):
        if op1 is None:
            P.op(eng, lambda e: e.tensor_scalar(out, in0, s1, None, op0), reads=reads, writes=writes)
        else:
            P.op(eng, lambda e: e.tensor_scalar(out, in0, s1, s2, op0, op1), reads=reads, writes=writes)

    def tt(out, in0, in1, op, reads, writes, eng="dve"):
        P.op(eng, lambda e: e.tensor_tensor(out, in0, in1, op), reads=reads, writes=writes)

    def stt(out, in0, sc, in1, op0, op1, reads, writes):
        P.op("dve", lambda e: e.scalar_tensor_tensor(out, in0, sc, in1, op0, op1), reads=reads, writes=writes)

    def cp(out, in_, reads, writes, eng="dve"):
        if eng == "act":
            P.op("act", lambda e: e.copy(out, in_), reads=reads, writes=writes)
        else:
            P.op(eng, lambda e: e.tensor_copy(out, in_), reads=reads, writes=writes)

    def ms(t, ap, val, eng="dve"):
        P.op(eng, lambda e: e.memset(ap, val), writes=[t])

    for t_, d_ in [(g1s, g1_d), (fbs, fb_d), (gfoxs, gfox_d), (cws, cw_d), (nA, alog_d), (dtbs, dtb_d), (ggdns, ggdn_d)]:
        P.dma("sp", lambda e, t_=t_, d_=d_: e.dma_start(out=t_[:], in_=d_), writes=[t_])
    P.dma("sp", lambda e: e.dma_start(out=cst[:], in_=cst_d), writes=[cst])
    P.dma("pool", lambda e: e.dma_start(out=cstb[:], in_=cst_d), writes=[cstb])
    for k in range(KD):
        P.dma("pool", lambda e, k=k: e.dma_start(out=WAb[:, k, :], in_=WA_d[:, k, :]), writes=[WAb])
    ms(ones_b, ones_b[:], 1.0)
    ms(ones_f, ones_f[:], 1.0)
    ms(epsc, epsc[:], EPS)
    ms(onec, onec[:], 1.0)
    ms(sel65, sel65[:], 0.0)
    ms(sel65, sel65[64:65, :], 1.0)
    ms(w64b, w64b[:], 1.0)
    ms(hmask, hmask[:], 0.0)
    ms(hmask, hmask[0:64, 0:1], 1.0)
    ms(hmask, hmask[64:128, 1:2], 1.0)
    ms(QA, QA[:], 0.0, "pool")
    ms(QB, QB[:], 0.0, "pool")
    ms(VA, VA[:], 1.0, "pool")
    ms(CV, CV[:], 0.0, "pool")
    ms(carry, carry[:], 0.0)
    for i in range(2):
        ms(S32[i], S32[i][:], 0.0)
        ms(Sb[i], Sb[i][:], 0.0)
    for i in range(NI):
        ms(gi[i]["vnew"], gi[i]["vnew"][:], 0.0, "pool")
    act(nA[:], nA[:], AF.Exp, [nA], [nA])
    ts(nA[:], nA[:], -1.0, None, ALU.mult, None, [nA], [nA])

    for j in range(NT):
        t0 = j * 512
        P.dma("sp", lambda e, t0=t0: e.dma_start(out=xt[:], in_=xT_d[:, :, t0:t0 + 512]), writes=[xt])
        pmt = pp[nxt("pp")]
        for k in range(KD):
            s = sqb[nxt("sqb")]
            act(s[:], xt[:, k, :], AF.Square, [xt], [s])
            mm(pmt[:], ones_b[:], s[:], k == 0, k == KD - 1, [s, ones_b], [pmt])
        act(rstd[:], pmt[:], AF.Ln, [pmt, epsc], [rstd], bias=epsc[:], scale=1.0 / D)
        act(rstd[:], rstd[:], AF.Exp, [rstd], [rstd], scale=-0.5)
        for k in range(KD):
            stt(hT[:, k, :], xt[:, k, :], g1s[:, k:k + 1], rstd[:], ALU.mult, ALU.mult, [xt, g1s, rstd], [hT])
        for grp in range(12):
            p = pp[nxt("pp")]
            for k in range(KD):
                mm(p[:], WAb[:, k, grp * 128:(grp + 1) * 128], hT[:, k, :], k == 0, k == KD - 1, [WAb, hT], [p])
            if grp < 2:
                cp(QA[0:64, grp, :], p[0:64, :], [p], [QA], "act")
                cp(QB[64:128, grp, :], p[64:128, :], [p], [QB], "act")
            elif grp < 4:
                cp(KT[:, grp - 2, t0:t0 + 512], p[:], [p], [KT], "act")
            elif grp < 10:
                cp(CV[:, grp - 4, 3:515], p[:], [p], [CV], "dve")
            else:
                act(gate[:, grp - 10, :], p[:], AF.Silu, [p], [gate])
        for s_ in range(4):
            p = pp[nxt("pp")]
            for k in range(KD):
                mm(p[:, 0:TMC], hT[:, k, s_ * 128:(s_ + 1) * 128], WAb[:, k, 1536:1536 + TMC], k == 0, k == KD - 1, [WAb, hT], [p])
            kt = 4 * j + s_
            for h4 in range(4):
                cp(VA[:, kt, h4, 0:64], p[:, h4 * 64:(h4 + 1) * 64], [p], [VA], "dve")
            cp(SM[:, s_, :], p[:, 256:264], [p], [SM], "dve")
            tt(tf[:, 0:4], SM[:, s_, 0:4], fbs[:], ALU.add, [SM, fbs], [tf])
            act(tf[:, 0:4], tf[:, 0:4], AF.Exp, [tf], [tf], scale=-1.0)
            act(tf[:, 0:4], tf[:, 0:4], AF.Ln, [tf, onec], [tf], bias=onec[:])
            pq = pg[nxt("pg", 2)]
            mm(pq[:, 0:4], tri_f, tf[:, 0:4], True, True, [cst, tf], [pq])
            mm(pq[:, 4:8], ones_f[:], tf[:, 0:4], True, True, [ones_f, tf], [pq])
            tt(cK[:, kt, :], pq[:, 0:4], carry[:], ALU.add, [pq, carry], [cK])
            tt(carry[:], pq[:, 4:8], carry[:], ALU.add, [pq, carry], [carry])
            if s_ == 1:
                cp(crefb[:], carry[:], [carry], [crefb])
            tt(tf[:, 4:6], SM[:, s_, 4:6], dtbs[:], ALU.add, [SM, dtbs], [tf])
            act(tf[:, 4:6], tf[:, 4:6], AF.Exp, [tf], [tf])
            act(tf[:, 4:6], tf[:, 4:6], AF.Ln, [tf, onec], [tf], bias=onec[:])
            tt(GB[:, s_, 0:2], tf[:, 4:6], nA[:], ALU.mult, [tf, nA], [GB])
            act(GB[:, s_, 2:4], SM[:, s_, 6:8], AF.Sigmoid, [SM], [GB])
            ts(GB[:, s_, 4:6], GB[:, s_, 2:4], -1.0, None, ALU.mult, None, [GB], [GB])

        def fox_gen(j=j, t0=t0):
            nkt = 4 * j + 4
            for h in range(4):
                pr, hp = h // 2, h % 2
                Qs = QA if hp == 0 else QB
                ts(biasJ[:, h, 0:nkt], cK[:, 0:nkt, h], crefb[:, h:h + 1], None, ALU.subtract, None, [cK, crefb], [biasJ])
                po = pot[0]

                def qk(kt):
                    dg_ = kt - 4 * j
                    qlo = 128 * dg_ if dg_ > 0 else 0
                    ps_ = pst[kt % 2]
                    mm(ps_[:, qlo:512], KT[:, pr, kt * 128:(kt + 1) * 128], Qs[:, pr, qlo:512], True, True, [KT, Qs], [ps_])
                    return ps_, qlo, dg_
                pend = {0: qk(0)}
                if nkt > 1:
                    pend[1] = qk(1)
                for kt in range(nkt):
                    ps_, qlo, dg_ = pend.pop(kt)
                    pt = PT[nxt("PT", 3)]
                    act(pt[:, qlo:512], ps_[:, qlo:512], AF.Exp, [ps_, biasJ], [pt], bias=biasJ[:, h, kt:kt + 1], scale=0.125)
                    if dg_ >= 0:
                        tt(pt[:, qlo:qlo + 128], pt[:, qlo:qlo + 128], tri_b, ALU.mult, [pt, cstb], [pt], eng="pool")
                    if kt + 2 < nkt:
                        pend[kt + 2] = qk(kt + 2)
                    mm(po[0:65, qlo:512], VA[:, kt, h, :], pt[:, qlo:512], kt == 0, kt == nkt - 1, [VA, pt], [po])
                    yield
                cp(sqf[0:65, :], po[0:65, :], [po], [sqf], "act")
                pn = pp[0]
                mm(pn[0:64, :], sel65[0:65, :], sqf[0:65, :], True, True, [sel65, sqf], [pn])
                P.op("dve", lambda e, pn=pn: e.reciprocal(rn[0:64, :], pn[0:64, :]), reads=[pn], writes=[rn])
                tt(sqf[0:64, :], sqf[0:64, :], rn[0:64, :], ALU.mult, [sqf, rn], [sqf])
                act(sqbf[0:64, :], sqf[0:64, :], AF.Square, [sqf], [sqbf])
                mm(pn[0:64, :], w64b[0:64, :], sqbf[0:64, :], True, True, [w64b, sqbf], [pn])
                act(rn[0:64, :], pn[0:64, :], AF.Ln, [pn, epsc], [rn], bias=epsc[0:64, :], scale=1.0 / 64)
                act(rn[0:64, :], rn[0:64, :], AF.Exp, [rn], [rn], scale=-0.5)
                fs = fo_st[nxt("fo")]
                stt(fs[0:64, :], sqf[0:64, :], gfoxs[:, h:h + 1], rn[0:64, :], ALU.mult, ALU.mult, [sqf, gfoxs, rn], [fs])
                P.dma("sp", lambda e, fs=fs, h=h, t0=t0: e.dma_start(out=mixT_d[h * 64:(h + 1) * 64, t0:t0 + 512], in_=fs[0:64, :]), reads=[fs])
                yield

        def gdn_gen(j=j, t0=t0):
            for i in range(6):
                c_ = cvt[0]
                ts(c_[:], CV[:, i, 0:512], cws[:, i * 4:i * 4 + 1], None, ALU.mult, None, [CV, cws], [c_])
                for k in range(1, 4):
                    stt(c_[:], CV[:, i, k:k + 512], cws[:, i * 4 + k:i * 4 + k + 1], c_[:], ALU.mult, ALU.add, [CV, cws, c_], [c_])
                if i % 3 == 2:
                    act(qkvn[:, i, :], c_[:], AF.Silu, [c_], [qkvn])
                else:
                    s_ = cvs[0]
                    act(s_[:], c_[:], AF.Silu, [c_], [s_])
                    q_ = sqb[nxt("sqb")]
                    act(q_[:], s_[:], AF.Square, [s_], [q_])
                    p = pp[1]
                    mm(p[:], ones_b[:], q_[:], True, True, [ones_b, q_], [p])
                    act(rstd[:], p[:], AF.Ln, [p, epsc], [rstd], bias=epsc[:])
                    act(rstd[:], rstd[:], AF.Exp, [rstd], [rstd], scale=-0.5)
                    stt(qkvn[:, i, :], s_[:], (128 ** -0.5) if i % 3 == 0 else 1.0, rstd[:], ALU.mult, ALU.mult, [s_, rstd], [qkvn])
                yield
            cp(CV[:, :, 0:3], CV[:, :, 512:515], [CV], [CV])

            for s_ in range(4):
                c0 = s_ * 128
                cs_ = slice(c0, c0 + 128)
                for hd in range(2):
                    G = gi[hd]
                    qT, kT, vT = qkvn[:, 3 * hd, cs_], qkvn[:, 3 * hd + 1, cs_], qkvn[:, 3 * hd + 2, cs_]
                    g_ = GB[:, s_, hd:hd + 1]
                    beta = GB[:, s_, 2 + hd:3 + hd]
                    nbeta = GB[:, s_, 4 + hd:5 + hd]
                    sc = G["sc"]
                    pq = pg[nxt("pg", 2)]
                    mm(pq[:, 0:1], triblk_f, g_, True, True, [cst, GB], [pq])
                    mm(pq[:, 1:2], onesblk_f, g_, True, True, [cst, GB], [pq])
                    cp(sc[:, 0:2], pq[:, 0:2], [pq], [sc])
                    ts(G["R"][:], triblk_f, g_, None, ALU.mult, None, [cst, GB], [G["R"]])
                    pq = pg[nxt("pg", 2)]
                    mm(pq[:], ones_f[:], G["R"][:], True, True, [ones_f, G["R"]], [pq])
                    ts(G["LT"][:], pq[:], sc[:, 0:1], 0.0, ALU.subtract, ALU.min, [pq, sc], [G["LT"]])
                    act(G["LT"][:], G["LT"][:], AF.Exp, [G["LT"]], [G["LT"]])
                    act(G["EB"][:], pq[:], AF.Exp, [pq], [G["EB"]])
                    tt(G["LTs"][:], G["LT"][:], strict_f, ALU.mult, [G["LT"], cst], [G["LTs"]], eng="pool")
                    tt(G["LTi"][:], G["LT"][:], triblk_f, ALU.mult, [G["LT"], cst], [G["LTi"]], eng="pool")
                    act(sc[:, 2:3], sc[:, 0:1], AF.Exp, [sc], [sc])
                    act(sc[:, 3:4], sc[:, 0:1], AF.Exp, [sc], [sc], bias=sc[:, 1:2], scale=-1.0)
                    ts(sc[:, 4:6], hmask[:], sc[:, 3:4], None, ALU.mult, None, [hmask, sc], [sc])
                    pk = pgb[nxt("pgb", 8)]
                    tr(pk[:], kT, ident_b, [qkvn, cstb], [pk])
                    ts(G["kgc"][:], pk[:], sc[:, 2:3], None, ALU.mult, None, [pk, sc], [G["kgc"]])
                    ts(G["kg0"][:], pk[:], sc[:, 4:5], None, ALU.mult, None, [pk, sc], [G["kg0"]])
                    ts(G["kg1"][:], pk[:], sc[:, 5:6], None, ALU.mult, None, [pk, sc], [G["kg1"]])
                    pv = pgb[nxt("pgb", 8)]
                    tr(pv[:], vT, ident_b, [qkvn, cstb], [pv])
                    cp(G["vtok"][:], pv[:], [pv], [G["vtok"]], "act")
                    pq = pg[nxt("pg", 2)]
                    mm(pq[:], kT, kT, True, True, [qkvn], [pq])
                    stt(G["X"][:], pq[:], nbeta, G["LTs"][:], ALU.mult, ALU.mult, [pq, GB, G["LTs"]], [G["X"]])
                    pq = pg[nxt("pg", 2)]
                    mm(pq[:], kT, qT, True, True, [qkvn], [pq])
                    tt(G["IT"][:], pq[:], G["LTi"][:], ALU.mult, [pq, G["LTi"]], [G["IT"]])
                    tt(G["qgT"][:], qT, G["EB"][:], ALU.mult, [qkvn, G["EB"]], [G["qgT"]])
                    pk = pgb[nxt("pgb", 8)]
                    tr(pk[:], G["X"][:], ident_b, [G["X"], cstb], [pk])
                    cp(G["XT"][:], pk[:], [pk], [G["XT"]], "act")
                    tt(G["Pm"][:], G["X"][:], ident_b, ALU.add, [G["X"], cstb], [G["Pm"]])
                    yield
                X = ["X", "X"]
                XTn_ = ["XT", "XT"]
                Pn_ = ["Pm", "Pm"]
                for lvl in range(5):
                    for hd in range(2):
                        G = gi[hd]
                        xa, xta = G[X[hd]], G[XTn_[hd]]
                        xb_n, xtb_n = ("Xn", "XTn") if X[hd] == "X" else ("X", "XT")
                        xb, xtb = G[xb_n], G[xtb_n]
                        pa_n = Pn_[hd]
                        pb_n = "Pn" if pa_n == "Pm" else "Pm"
                        pq = pg[nxt("pg", 2)]
                        mm(pq[:], xta[:], xa[:], True, True, [xta, xa], [pq])
                        cp(xb[:], pq[:], [pq], [xb], "act")
                        pq2 = pg[nxt("pg", 2)]
                        mm(pq2[:], xa[:], xta[:], True, True, [xta, xa], [pq2])
                        cp(xtb[:], pq2[:], [pq2], [xtb], "dve")
                        pq3 = pg[nxt("pg", 2)]
                        mm(pq3[:], xtb[:], G[pa_n][:], True, True, [xtb, G[pa_n]], [pq3])
                        tt(G[pb_n][:], pq3[:], G[pa_n][:], ALU.add, [pq3, G[pa_n]], [G[pb_n]])
                        X[hd], XTn_[hd], Pn_[hd] = xb_n, xtb_n, pb_n
                        yield
                for hd in range(2):
                    G = gi[hd]
                    Pf = G[Pn_[hd]]
                    pq = pg[nxt("pg", 2)]
                    mm(pq[:], G["kgc"][:], Pf[:], True, True, [G["kgc"], Pf], [pq])
                    ts(G["nwT"][:], pq[:], -1.0, None, ALU.mult, None, [pq], [G["nwT"]])
                po_ = [pov[0], pov[1]]
                for X_ in range(2):
                    rs_ = slice(64 * X_, 64 * X_ + 64)
                    for hd in range(2):
                        G = gi[hd]
                        Pf = G[Pn_[hd]]
                        beta = GB[:, s_, 2 + hd:3 + hd]
                        pq = pg[nxt("pg", 2)]
                        mm(pq[:], Pf[:], G["vtok"][:], True, False, [Pf, G["vtok"]], [pq])
                        mm(pq[:], G["nwT"][:], Sb[hd][:], False, True, [G["nwT"], Sb[hd]], [pq])
                        ts(G["vnew"][rs_, :], pq[rs_, :], GB[rs_, s_, 2 + hd:3 + hd], None, ALU.mult, None, [pq, GB], [G["vnew"]])
                        pq2 = po_[hd]
                        mm(pq2[:, rs_], Sb[hd][:], G["qgT"][:, rs_], True, False, [Sb[hd], G["qgT"]], [pq2])
                        mm(pq2[:, rs_], G["vnew"][:], G["IT"][:, rs_], False, True, [G["vnew"], G["IT"]], [pq2])
                        pq3 = pg[nxt("pg", 2)]
                        kgx = G["kg0"] if X_ == 0 else G["kg1"]
                        mm(pq3[:], kgx[:], G["vnew"][:], True, True, [kgx, G["vnew"]], [pq3])
                        stt(S32[hd][:], S32[hd][:], G["EB"][:, 64 * X_ + 63:64 * X_ + 64], pq3[:], ALU.mult, ALU.add, [S32[hd], G["EB"], pq3], [S32[hd]])
                        cp(Sb[hd][:], S32[hd][:], [S32[hd]], [Sb[hd]], "act")
                        yield
                for hd in range(2):
                    cp(OT[hd][:, cs_], po_[hd][:], [po_[hd]], [OT[hd]], "act")
            for hd in range(2):
                q_ = sqb[nxt("sqb")]
                act(q_[:], OT[hd][:], AF.Square, [OT[hd]], [q_])
                p = pp[1]
                mm(p[:], ones_b[:], q_[:], True, True, [ones_b, q_], [p])
                act(rstd[:], p[:], AF.Ln, [p, epsc], [rstd], bias=epsc[:], scale=1.0 / 128)
                act(rstd[:], rstd[:], AF.Exp, [rstd], [rstd], scale=-0.5)
                gs = OT[hd]
                stt(gs[:], OT[hd][:], ggdns[:, 0:1], rstd[:], ALU.mult, ALU.mult, [OT[hd], ggdns, rstd], [gs])
                tt(gs[:], gs[:], gate[:, hd, :], ALU.mult, [gs, gate], [gs])
                P.dma("sp", lambda e, gs=gs, hd=hd, t0=t0: e.dma_start(out=mixT_d[256 + hd * 128:256 + (hd + 1) * 128, t0:t0 + 512], in_=gs[:]), reads=[gs])

        gf_, gg_ = fox_gen(), gdn_gen()
        n_f, n_g = 4 * (4 * j + 4 + 1), 70
        acc = 0.0
        alive_f, alive_g = True, True
        while alive_f or alive_g:
            if alive_g:
                try:
                    next(gg_)
                except StopIteration:
                    alive_g = False
            acc += n_f / n_g
            while alive_f and (acc >= 1.0 or not alive_g):
                acc -= 1.0
                try:
                    next(gf_)
                except StopIteration:
                    alive_f = False
    evs = []
    for t_ in fo_st + OT:
        evs += list(t_.d.rs)
    P.finish("sp", evs)
    P.emit()
    return nc


D = 1024
KD = 8
EPS = 1e-6


def build_B(NTOK, TG, E, F, moe, final):
    nc = bass.Bass("TRN2", target_bir_lowering=False)
    P = Prog(nc)
    NF = F // 128
    NTT = TG // 512
    NG = NTOK // TG
    dt = nc.dram_tensor
    xT_d = dt("xT", [D, NTOK], F32, kind="ExternalInput").ap().rearrange("(k p) n -> p k n", p=128)
    mixT_d = dt("mixT", [D, NTOK], F32, kind="ExternalInput").ap().rearrange("(k p) n -> p k n", p=128)
    wout_d = dt("wout", [D, D], F32, kind="ExternalInput").ap().rearrange("(k p) n -> p k n", p=128)
    g2_d = dt("g2", [128, KD], F32, kind="ExternalInput").ap()
    gf_d = dt("gf", [128, KD], F32, kind="ExternalInput").ap()
    w1_d = dt("w1", [E, D, F], F32, kind="ExternalInput").ap()
    w3_d = dt("w3", [E, D, F], F32, kind="ExternalInput").ap()
    w2_d = dt("w2", [E, F, D], F32, kind="ExternalInput").ap()
    wr_d = dt("wr", [D, 8], F32, kind="ExternalInput").ap().rearrange("(k p) n -> p k n", p=128)
    id_d = dt("ident", [128, 128], F32, kind="ExternalInput").ap()
    yT_d = dt("yT", [D, NTOK], F32, kind="ExternalOutput").ap().rearrange("(k p) n -> p k n", p=128)
    if final:
        yfT_d = dt("yfT", [D, NTOK], F32, kind="ExternalOutput").ap().rearrange("(k p) n -> p k n", p=128)

    xs = P.sb("xs", [128, KD, TG], F32)
    hT = P.sb("hT", [128, KD, TG], BF16)
    gT = P.sb("gT", [128, NF, TG], BF16)
    mixs = gT
    woutb = P.sb("woutb", [128, KD, D], BF16)
    w1b = [P.sb("w1b%d" % i, [128, KD, 256], BF16) for i in range(2)]
    w3b = [P.sb("w3b%d" % i, [128, KD, 256], BF16) for i in range(2)]
    w2b = [P.sb("w2b%d" % i, [128, NF, 128], BF16) for i in range(2)]
    g2s = P.sb("g2s", [128, KD], F32)
    gfs = P.sb("gfs", [128, KD], F32)
    ident = P.sb("ident_s", [128, 128], F32)
    ones_b = P.sb("ones_b", [128, 128], BF16)
    ones_f = P.sb("ones_f", [128, 128], F32)
    epsc = P.sb("epsc", [128, 1], F32)
    sq = [P.sb("sq%d" % i, [128, 512], BF16) for i in range(2)]
    rstd = P.sb("rstd", [128, 512], F32)
    sil = [P.sb("sil%d" % i, [128, 512], F32) for i in range(2)]
    tmpb = [P.sb("tmpb%d" % i, [128, 512], F32) for i in range(2)]
    if moe:
        h32 = [P.sb("h32_%d" % i, [128, 512], F32) for i in range(2)]
        wrs = P.sb("wrs", [128, KD, 8], F32)
        lg = P.sb("lg", [128, 8], F32)
        lgT = P.sb("lgT", [8, 512], F32)
        top8 = P.sb("top8", [128, 8], F32)
        rw = P.sb("rw", [128, TG // 128, 8], F32)
        tmp8 = P.sb("tmp8", [128, 8], F32)
        tmp1 = P.sb("tmp1", [128, 2], F32)
        dg = [P.sb("dg%d" % i, [128, 128], F32) for i in range(2)]
        wbc = P.sb("wbc", [128, TG], F32)
    pa = [P.ps("pa%d" % i, [128, 512], F32) for i in range(2)]
    pb = [P.ps("pb%d" % i, [128, 512], F32) for i in range(2)]
    py = [P.ps("py%d" % i, [128, 512], F32) for i in range(2)]
    pm = [P.ps("pm%d" % i, [128, 512], F32) for i in range(2)]

    P.dma("sp", lambda e: e.dma_start(out=ident[:], in_=id_d), writes=[ident])
    P.dma("sp", lambda e: e.dma_start(out=g2s[:], in_=g2_d), writes=[g2s])
    P.dma("sp", lambda e: e.dma_start(out=gfs[:], in_=gf_d), writes=[gfs])
    P.op("dve", lambda e: e.memset(ones_b[:], 1.0), writes=[ones_b])
    P.op("dve", lambda e: e.memset(ones_f[:], 1.0), writes=[ones_f])
    P.op("dve", lambda e: e.memset(epsc[:], EPS), writes=[epsc])
    for k in range(KD):
        P.dma("pool", lambda e, k=k: e.dma_start(out=woutb[:, k, :], in_=wout_d[:, k, :]), writes=[woutb])
    if moe:
        P.dma("sp", lambda e: e.dma_start(out=wrs[:], in_=wr_d), writes=[wrs])

    cnt = {"sq": 0, "pm": 0, "pa": 0, "py": 0, "w13": 0, "w2": 0, "sil": 0, "dg": 0, "h32": 0}

    def nxt(key, n=2):
        v = cnt[key] % n
        cnt[key] += 1
        return v

    def rmsnorm(src, gs, dst_bf, tt, dst32=None):
        sl = slice(tt * 512, (tt + 1) * 512)
        pmt = pm[nxt("pm")]
        for k in range(KD):
            s = sq[nxt("sq")]
            P.op("act", lambda e, s=s, k=k: e.activation(s[:], src[:, k, sl], AF.Square), reads=[src], writes=[s])
            P.op("pe", lambda e, s=s, k=k: e.matmul(pmt[:], lhsT=ones_b[:], rhs=s[:], start=(k == 0), stop=(k == KD - 1)),
                 reads=[s, ones_b], writes=[pmt])
        P.op("act", lambda e: e.activation(rstd[:], pmt[:], AF.Sqrt, bias=epsc[:], scale=1.0 / D), reads=[pmt, epsc], writes=[rstd])
        P.op("dve", lambda e: e.reciprocal(rstd[:], rstd[:]), reads=[rstd], writes=[rstd])
        for k in range(KD):
            P.op("dve", lambda e, k=k: e.scalar_tensor_tensor(dst_bf[:, k, sl], src[:, k, sl], gs[:, k:k + 1], rstd[:], ALU.mult, ALU.mult),
                 reads=[src, gs, rstd], writes=[dst_bf])

    for gi in range(NG):
        t0 = gi * TG
        P.dma("sp", lambda e, t0=t0: e.dma_start(out=xs[:], in_=xT_d[:, :, t0:t0 + TG]), writes=[xs])
        for k in range(KD):
            P.dma("pool", lambda e, t0=t0, k=k: e.dma_start(out=mixs[:, k, :], in_=mixT_d[:, k, t0:t0 + TG]), writes=[mixs])
        for g in range(KD):
            for tt in range(NTT):
                sl = slice(tt * 512, (tt + 1) * 512)
                p = py[nxt("py")]
                for k in range(KD):
                    P.op("pe", lambda e, p=p, k=k, g=g, sl=sl: e.matmul(p[:], lhsT=woutb[:, k, g * 128:(g + 1) * 128], rhs=mixs[:, k, sl],
                                                                     start=(k == 0), stop=(k == KD - 1)), reads=[woutb, mixs], writes=[p])
                P.op("dve", lambda e, p=p, g=g, sl=sl: e.tensor_tensor(xs[:, g, sl], xs[:, g, sl], p[:], ALU.add), reads=[xs, p], writes=[xs])
        for tt in range(NTT):
            rmsnorm(xs, g2s, hT, tt)
            if moe:
                sl_ = slice(tt * 512, (tt + 1) * 512)
                prt = pm[nxt("pm")]
                for k in range(KD):
                    hk = h32[nxt("h32")]
                    P.op("dve", lambda e, k=k, hk=hk, sl_=sl_: e.scalar_tensor_tensor(hk[:], xs[:, k, sl_], g2s[:, k:k + 1], rstd[:], ALU.mult, ALU.mult),
                         reads=[xs, g2s, rstd], writes=[hk])
                    P.op("pe", lambda e, k=k, hk=hk, prt=prt: e.matmul(prt[0:8, :], lhsT=wrs[:, k, :], rhs=hk[:], start=(k == 0), stop=(k == KD - 1)),
                         reads=[hk, wrs], writes=[prt])
                P.op("act", lambda e, prt=prt: e.copy(lgT[:], prt[0:8, :]), reads=[prt], writes=[lgT])
                prt = pm[nxt("pm")]
                for ts in range(4):
                    P.op("pe", lambda e, ts=ts, prt=prt: e.transpose(prt[:, ts * 8:(ts + 1) * 8], lgT[0:8, ts * 128:(ts + 1) * 128], ident[0:8, 0:8]),
                         reads=[lgT, ident], writes=[prt])
                for ts in range(4):
                    st = tt * 4 + ts
                    p = prt
                    P.op("dve", lambda e, p=p, ts=ts: e.tensor_copy(lg[:], p[:, ts * 8:(ts + 1) * 8]), reads=[p], writes=[lg])
                    P.op("dve", lambda e: e.max(out=top8[:], in_=lg[:]), reads=[lg], writes=[top8])
                    P.op("dve", lambda e: e.tensor_scalar(tmp1[:, 0:1], top8[:, 0:1], -1.0, None, ALU.mult), reads=[top8], writes=[tmp1])
                    P.op("act", lambda e: e.activation(tmp8[:], lg[:], AF.Exp, bias=tmp1[:, 0:1], scale=1.0), reads=[lg, tmp1], writes=[tmp8])
                    P.op("act", lambda e: e.activation(tmp1[:, 1:2], top8[:, 1:2], AF.Exp, bias=tmp1[:, 0:1], scale=1.0), reads=[top8, tmp1], writes=[tmp1])
                    P.op("dve", lambda e: e.tensor_scalar(tmp1[:, 1:2], tmp1[:, 1:2], 1.0, None, ALU.add), reads=[tmp1], writes=[tmp1])
                    P.op("dve", lambda e: e.reciprocal(tmp1[:, 1:2], tmp1[:, 1:2]), reads=[tmp1], writes=[tmp1])
                    P.op("dve", lambda e: e.tensor_scalar(lg[:], lg[:], top8[:, 1:2], None, ALU.is_ge), reads=[lg, top8], writes=[lg])
                    P.op("dve", lambda e, st=st: e.scalar_tensor_tensor(rw[:, st, :], tmp8[:], tmp1[:, 1:2], lg[:], ALU.mult, ALU.mult),
                         reads=[tmp8, tmp1, lg], writes=[rw])
        tasks = []
        for ex in range(E):
            for fp in range(NF // 2):
                tasks.append(("h", ex, fp))
            for g in range(KD):
                tasks.append(("y", ex, g))
        bufs = {}

        def issue(i):
            kind, ex, j = tasks[i]
            if kind == "h":
                wi = nxt("w13")
                a_, b_ = w1b[wi], w3b[wi]
                bufs[i] = (a_, b_)
                f0 = j * 256
                src1 = w1_d[ex].rearrange("(k p) f -> p k f", p=128)
                src3 = w3_d[ex].rearrange("(k p) f -> p k f", p=128)
                for k in range(KD):
                    P.dma("pool", lambda e, a_=a_, k=k, f0=f0, src1=src1: e.dma_start(out=a_[:, k, :], in_=src1[:, k, f0:f0 + 256]), writes=[a_])
                    P.dma("pool", lambda e, b_=b_, k=k, f0=f0, src3=src3: e.dma_start(out=b_[:, k, :], in_=src3[:, k, f0:f0 + 256]), writes=[b_])
            else:
                w_ = w2b[nxt("w2")]
                bufs[i] = w_
                src2 = w2_d[ex].rearrange("(c p) d -> p c d", p=128)
                P.dma("pool", lambda e, w_=w_, j=j, src2=src2: e.dma_start(out=w_[:], in_=src2[:, :, j * 128:(j + 1) * 128]), writes=[w_])

        issue(0)
        for i, (kind, ex, j) in enumerate(tasks):
            if i + 1 < len(tasks):
                issue(i + 1)
            if kind == "h" and j == 0 and moe:
                for st in range(TG // 128):
                    d_ = dg[nxt("dg")]
                    P.op("dve", lambda e, d_=d_, st=st, ex=ex: e.tensor_scalar(d_[:], ident[:], rw[:, st, ex:ex + 1], None, ALU.mult),
                         reads=[ident, rw], writes=[d_])
                    p = pm[nxt("pm")]
                    P.op("pe", lambda e, p=p, d_=d_: e.matmul(p[:, 0:128], lhsT=ones_f[:], rhs=d_[:], start=True, stop=True), reads=[ones_f, d_], writes=[p])
                    P.op("act", lambda e, p=p, st=st: e.copy(wbc[:, st * 128:(st + 1) * 128], p[:, 0:128]), reads=[p], writes=[wbc])
            if kind == "h":
                a_, b_ = bufs[i]
                for fc in range(2):
                    f = j * 2 + fc
                    for tt in range(NTT):
                        sl = slice(tt * 512, (tt + 1) * 512)
                        pi = nxt("pa")
                        p_a, p_b = pa[pi], pb[pi]
                        for k in range(KD):
                            P.op("pe", lambda e, p_a=p_a, a_=a_, k=k, fc=fc, sl=sl: e.matmul(p_a[:], lhsT=a_[:, k, fc * 128:(fc + 1) * 128], rhs=hT[:, k, sl],
                                                                                      start=(k == 0), stop=(k == KD - 1)), reads=[a_, hT], writes=[p_a])
                        for k in range(KD):
                            P.op("pe", lambda e, p_b=p_b, b_=b_, k=k, fc=fc, sl=sl: e.matmul(p_b[:], lhsT=b_[:, k, fc * 128:(fc + 1) * 128], rhs=hT[:, k, sl],
                                                                                      start=(k == 0), stop=(k == KD - 1)), reads=[b_, hT], writes=[p_b])
                        s_ = sil[nxt("sil")]
                        P.op("act", lambda e, s_=s_, p_a=p_a: e.activation(s_[:], p_a[:], AF.Silu), reads=[p_a], writes=[s_])
                        if moe:
                            t_ = tmpb[pi]
                            P.op("dve", lambda e, t_=t_, p_b=p_b, sl=sl: e.tensor_tensor(t_[:], p_b[:], wbc[:, sl], ALU.mult), reads=[p_b, wbc], writes=[t_])
                            P.op("dve", lambda e, t_=t_, s_=s_, f=f, sl=sl: e.tensor_tensor(gT[:, f, sl], s_[:], t_[:], ALU.mult), reads=[s_, t_], writes=[gT])
                        else:
                            P.op("dve", lambda e, s_=s_, p_b=p_b, f=f, sl=sl: e.tensor_tensor(gT[:, f, sl], s_[:], p_b[:], ALU.mult), reads=[s_, p_b], writes=[gT])
            else:
                w_ = bufs[i]
                g = j
                for tt in range(NTT):
                    sl = slice(tt * 512, (tt + 1) * 512)
                    p = py[nxt("py")]
                    for f in range(NF):
                        P.op("pe", lambda e, p=p, w_=w_, f=f, sl=sl: e.matmul(p[:], lhsT=w_[:, f, :], rhs=gT[:, f, sl], start=(f == 0), stop=(f == NF - 1)),
                             reads=[w_, gT], writes=[p])
                    P.op("dve", lambda e, p=p, g=g, sl=sl: e.tensor_tensor(xs[:, g, sl], xs[:, g, sl], p[:], ALU.add), reads=[xs, p], writes=[xs])
        P.dma("sp", lambda e, t0=t0: e.dma_start(out=yT_d[:, :, t0:t0 + TG], in_=xs[:]), reads=[xs])
        if final:
            for tt in range(NTT):
                rmsnorm(xs, gfs, xs, tt)
            P.dma("sp", lambda e, t0=t0: e.dma_start(out=yfT_d[:, :, t0:t0 + TG], in_=xs[:]), reads=[xs])
    P.finish("sp", list(xs.d.rs))
    P.emit()
    return nc


from concourse.bass_utils import run_bass_kernel_spmd

SEQ = 8192
BATCH = 4
DEPTH = 4
_PROGS = {}


def _prog(key):
    if key not in _PROGS:
        if key == "A":
            _PROGS[key] = build_A(SEQ)
        elif key == "Bd":
            _PROGS[key] = build_B(SEQ // 2, 1024, 1, 2816, False, False)
        else:
            _PROGS[key] = build_B(SEQ // 2, 1024, 8, 3584, True, True)
    return _PROGS[key]


def kernel(x, ln1_g, w_in, fox_f_bias, fox_norm_g, gdn_conv_w, gdn_a_log, gdn_dt_bias,
           gdn_norm_g, w_out, ln2_g, ffn_w1, ffn_w3, ffn_w2, router_w, exp_w1, exp_w3,
           exp_w2, final_g):
    f32 = np.float32
    x = np.asarray(x, f32)
    xT = [np.ascontiguousarray(x[b].T) for b in range(BATCH)]
    ident = np.eye(128, dtype=f32)
    gf = lay8(np.asarray(final_g, f32))
    cores = list(range(8))
    H = SEQ // 2
    outT = None
    for layer in range(DEPTH):
        maps = []
        for b in range(BATCH):
            for hh in range(2):
                maps.append(prep_A(xT[b], np.asarray(ln1_g[layer], f32), np.asarray(w_in[layer], f32),
                                   np.asarray(fox_f_bias[layer], f32), np.asarray(fox_norm_g[layer], f32),
                                   np.asarray(gdn_conv_w[layer], f32), np.asarray(gdn_a_log[layer], f32),
                                   np.asarray(gdn_dt_bias[layer], f32), np.asarray(gdn_norm_g[layer], f32), hh))
        resA = run_bass_kernel_spmd(_prog("A"), maps, core_ids=cores).results
        mixT = []
        for b in range(BATCH):
            m0, m1 = np.asarray(resA[2 * b]["mixT"]), np.asarray(resA[2 * b + 1]["mixT"])
            mixT.append(np.concatenate([m0[0:256], m1[0:256], m0[256:512], m1[256:512]], axis=0))
        j = layer // 2
        moe = (layer % 2 == 1)
        maps = []
        for b in range(BATCH):
            for th in range(2):
                sl = slice(th * H, (th + 1) * H)
                m = {"xT": np.ascontiguousarray(xT[b][:, sl]), "mixT": np.ascontiguousarray(mixT[b][:, sl]),
                     "wout": np.asarray(w_out[layer], f32), "g2": lay8(np.asarray(ln2_g[layer], f32)), "gf": gf, "ident": ident}
                if moe:
                    m.update({"w1": np.asarray(exp_w1[j], f32), "w3": np.asarray(exp_w3[j], f32), "w2": np.asarray(exp_w2[j], f32),
                              "wr": np.asarray(router_w[j], f32)})
                else:
                    m.update({"w1": np.asarray(ffn_w1[j], f32)[None], "w3": np.asarray(ffn_w3[j], f32)[None],
                              "w2": np.asarray(ffn_w2[j], f32)[None], "wr": np.zeros((1024, 8), f32)})
                maps.append(m)
        resB = run_bass_kernel_spmd(_prog("Bm" if moe else "Bd"), maps, core_ids=cores).results
        for b in range(BATCH):
            xT[b] = np.concatenate([np.asarray(resB[2 * b]["yT"]), np.asarray(resB[2 * b + 1]["yT"])], axis=1)
        if layer == DEPTH - 1:
            outT = [np.concatenate([np.asarray(resB[2 * b]["yfT"]), np.asarray(resB[2 * b + 1]["yfT"])], axis=1) for b in range(BATCH)]
    out = np.stack([np.ascontiguousarray(o.T) for o in outT], axis=0).astype(f32)
    return out
```

```python
from contextlib import ExitStack
import numpy as np
import concourse.bass as bass
import concourse.mybir as mybir

F32 = mybir.dt.float32
BF16 = mybir.dt.bfloat16
AF = mybir.ActivationFunctionType
ALU = mybir.AluOpType
AX = mybir.AxisListType

SAME_ENGINE_SYNC = True


class Dep:
    __slots__ = ("w", "rs", "wneeds", "dsem", "name")

    def __init__(self, name=""):
        self.w = None
        self.rs = []
        self.wneeds = []
        self.dsem = None
        self.name = name


class T:
    def __init__(self, h, name, nsub=1):
        self.h = h
        self.name = name
        self.d = Dep(name)
        self.subs = [Dep(name + str(i)) for i in range(nsub)] if nsub > 1 else None

    def __getitem__(self, idx):
        return self.h[idx]


class Prog:
    ENGS = ["pe", "act", "dve", "pool", "sp"]

    def __init__(self, nc):
        self.nc = nc
        self.items = {e: [] for e in self.ENGS}
        self.cnt = {e: 0 for e in self.ENGS}
        self.known = {e: {} for e in self.ENGS}
        self.dsems = []
        self.dcnt = {}
        self.stack = ExitStack()
        self.final_events = []

    def sb(self, name, shape, dtype, nsub=1):
        h = self.stack.enter_context(self.nc.sbuf_tensor(name, list(shape), dtype))
        return T(h, name, nsub)

    def ps(self, name, shape, dtype=F32, nsub=1):
        h = self.stack.enter_context(self.nc.psum_tensor(name, list(shape), dtype))
        return T(h, name, nsub)

    def _need(self, eng, ev, waits):
        if ev is None:
            return
        key, val = ev
        if key == eng and (eng == "pe" or not SAME_ENGINE_SYNC):
            return
        if self.known[eng].get(key, 0) >= val:
            return
        if waits.get(key, 0) < val:
            waits[key] = val

    def _collect(self, eng, reads, writes):
        waits = {}
        for d in reads:
            self._need(eng, d.w, waits)
        wn = {}
        for d in writes:
            evs = [d.w] + list(d.rs)
            for ev in evs:
                self._need(eng, ev, waits)
        for k, v in waits.items():
            self.known[eng][k] = v
        return waits

    def _commit(self, ev, reads, writes):
        for d in reads:
            d.rs.append(ev)
        for d in writes:
            d.w = ev
            d.rs = []

    @staticmethod
    def _deps(lst):
        out = []
        for x in lst:
            if x is None:
                continue
            if isinstance(x, T):
                out.append(x.d)
            else:
                out.append(x)
        return out

    def op(self, eng, fn, reads=(), writes=()):
        reads = self._deps(reads)
        writes = self._deps(writes)
        waits = self._collect(eng, reads, writes)
        self.cnt[eng] += 1
        ev = (eng, self.cnt[eng])
        self._commit(ev, reads, writes)
        self.items[eng].append((fn, waits, eng, 1))

    def dma(self, q, fn, reads=(), writes=(), semdep=None):
        reads = self._deps(reads)
        writes = self._deps(writes)
        if semdep is None:
            semdep = writes[0] if writes else reads[0]
        elif isinstance(semdep, T):
            semdep = semdep.d
        if semdep.dsem is None:
            semdep.dsem = ("dma", len(self.dsems))
            self.dsems.append(semdep)
            self.dcnt[semdep.dsem] = 0
        key = semdep.dsem
        waits = {}
        for d in reads:
            self._need(q, d.w, waits)
        for d in writes:
            evs = list(d.rs)
            if d.w is not None and not (d.w[0] == key and not d.rs):
                evs.append(d.w)
            for ev in evs + (d.wneeds if (d.w is not None and d.w[0] == key and not d.rs) else []):
                self._need(q, ev, waits)
            if not (d.w is not None and d.w[0] == key and not d.rs):
                d.wneeds = evs
        for k, v in waits.items():
            self.known[q][k] = v
        self.dcnt[key] += 16
        ev = (key, self.dcnt[key])
        self._commit(ev, reads, writes)
        self.items[q].append((fn, waits, key, 16))
        return ev

    def finish(self, eng, events):
        waits = {}
        for ev in events:
            self._need(eng, ev, waits)
        self.items[eng].append((None, waits, None, 0))

    def emit(self):
        nc = self.nc
        with ExitStack() as st:
            sems = {}
            for e in self.ENGS:
                sems[e] = st.enter_context(nc.semaphore("s_" + e))
            for i, d in enumerate(self.dsems):
                sems[d.dsem] = st.enter_context(nc.semaphore("d%d" % i))
            block = st.enter_context(nc.Block())

            def replay(ename):
                def f(eng):
                    for fn, waits, inckey, incv in self.items[ename]:
                        for k, v in waits.items():
                            eng.wait_ge(sems[k], v)
                        if fn is not None:
                            ins = fn(eng)
                            ins.then_inc(sems[inckey], incv)
                return f

            block.tensor(replay("pe"))
            block.scalar(replay("act"))
            block.vector(replay("dve"))
            block.gpsimd(replay("pool"))
            block.sync(replay("sp"))
        self.stack.close()


FOX_W = 512
def wa_cols(hh):
    fq, fk, fv, ff = 0, 512, 1024, 1536
    gq, gk, gv = 1544, 1544 + 512, 1544 + 1024
    ga, gb, gz = 3080, 3084, 3088
    heads = [4 * hh + i for i in range(4)]
    gh = [2 * hh, 2 * hh + 1]
    idx = []
    for base in (fq, fk):
        for h in heads:
            idx += list(range(base + h * 64, base + (h + 1) * 64))
    for g in gh:
        for base in (gq, gk, gv):
            idx += list(range(base + g * 128, base + (g + 1) * 128))
    for g in gh:
        idx += list(range(gz + g * 128, gz + (g + 1) * 128))
    for h in heads:
        idx += list(range(fv + h * 64, fv + (h + 1) * 64))
    idx += [ff + h for h in heads]
    idx += [ga + g for g in gh]
    idx += [gb + g for g in gh]
    assert len(idx) == 1800
    return np.array(idx)

def consts():
    i = np.arange(128)
    ident = np.eye(128, dtype=np.float32)
    tri = (i[:, None] <= i[None, :]).astype(np.float32)
    same = (i[:, None] // 64 == i[None, :] // 64)
    triblk = (tri > 0) & same
    strict = (i[:, None] < i[None, :]) & same
    c = np.stack([ident, tri, triblk.astype(np.float32), strict.astype(np.float32), same.astype(np.float32)], axis=1)
    return np.ascontiguousarray(c.astype(np.float32))

def lay8(g):
    return np.ascontiguousarray(g.reshape(8, 128).T)

def prep_A(xTb, ln1_g, w_in, fox_f_bias, fox_norm_g, conv_w, a_log, dt_bias, gdn_norm_g, hh):
    heads = [4 * hh + i for i in range(4)]
    gh = [2 * hh, 2 * hh + 1]
    cw = np.zeros((128, 24), np.float32)
    for g_i, g in enumerate(gh):
        for m in range(3):
            ch0 = m * 512 + g * 128
            cw[:, (g_i * 3 + m) * 4:(g_i * 3 + m) * 4 + 4] = conv_w[:, ch0:ch0 + 128].T
    return {
        "xT": xTb,
        "WA": np.ascontiguousarray(w_in[:, wa_cols(hh)]),
        "g1": lay8(ln1_g),
        "fbias": np.ascontiguousarray(np.broadcast_to(fox_f_bias[heads][None, :], (128, 4))),
        "gfox": np.ascontiguousarray(fox_norm_g.reshape(8, 64)[heads].T),
        "convw": cw,
        "alog": np.ascontiguousarray(np.broadcast_to(a_log[gh][None, :], (128, 2))),
        "dtb": np.ascontiguousarray(np.broadcast_to(dt_bias[gh][None, :], (128, 2))),
        "ggdn": np.ascontiguousarray(gdn_norm_g.reshape(128, 1)),
        "cst": consts(),
    }


D = 1024
KD = 8
EPS = 1e-6
NCOL = 1800
TMC = 264


def build_A(TLEN):
    nc = bass.Bass("TRN2", target_bir_lowering=False)
    P = Prog(nc)
    NT = TLEN // 512
    NKT = TLEN // 128
    dt = nc.dram_tensor
    xT_d = dt("xT", [D, TLEN], F32, kind="ExternalInput").ap().rearrange("(k p) n -> p k n", p=128)
    WA_d = dt("WA", [D, NCOL], F32, kind="ExternalInput").ap().rearrange("(k p) n -> p k n", p=128)
    g1_d = dt("g1", [128, KD], F32, kind="ExternalInput").ap()
    fb_d = dt("fbias", [128, 4], F32, kind="ExternalInput").ap()
    gfox_d = dt("gfox", [64, 4], F32, kind="ExternalInput").ap()
    cw_d = dt("convw", [128, 24], F32, kind="ExternalInput").ap()
    alog_d = dt("alog", [128, 2], F32, kind="ExternalInput").ap()
    dtb_d = dt("dtb", [128, 2], F32, kind="ExternalInput").ap()
    ggdn_d = dt("ggdn", [128, 1], F32, kind="ExternalInput").ap()
    cst_d = dt("cst", [128, 5, 128], F32, kind="ExternalInput").ap()
    mixT_d = dt("mixT", [512, TLEN], F32, kind="ExternalOutput").ap()

    sb = P.sb
    WAb = sb("WAb", [128, KD, NCOL], BF16)
    xt = sb("xt", [128, KD, 512], F32)
    hT = sb("hT", [128, KD, 512], BF16)
    KT = sb("KT", [128, 2, TLEN], BF16)
    VA = sb("VA", [128, NKT, 4, 65], BF16)
    QA = sb("QA", [128, 2, 512], BF16)
    QB = sb("QB", [128, 2, 512], BF16)
    cK = sb("cK", [128, NKT, 4], F32)
    biasJ = sb("biasJ", [128, 4, NKT], F32)
    carry = sb("carry", [128, 4], F32)
    crefb = sb("crefb", [128, 4], F32)
    SM = sb("SM", [128, 4, 8], F32)
    GB = sb("GB", [128, 4, 6], F32)
    tf = sb("tf", [128, 8], F32)
    CV = sb("CV", [128, 6, 515], F32)
    cvt = [sb("cvt%d" % i, [128, 512], F32) for i in range(1)]
    cvs = [sb("cvs%d" % i, [128, 512], F32) for i in range(1)]
    qkvn = sb("qkvn", [128, 6, 512], BF16)
    gate = sb("gate", [128, 2, 512], BF16)
    PT = [sb("PT%d" % i, [128, 512], BF16) for i in range(3)]
    sqb = [sb("sqb%d" % i, [128, 512], BF16) for i in range(2)]
    sqf = sb("sqf", [128, 512], F32)
    fo_st = [sb("fo_st%d" % i, [128, 512], F32) for i in range(2)]
    OT = [sb("OT%d" % i, [128, 512], F32) for i in range(2)]
    g1s = sb("g1s", [128, KD], F32)
    fbs = sb("fbs", [128, 4], F32)
    gfoxs = sb("gfoxs", [64, 4], F32)
    cws = sb("cws", [128, 24], F32)
    nA = sb("nA", [128, 2], F32)
    dtbs = sb("dtbs", [128, 2], F32)
    ggdns = sb("ggdns", [128, 1], F32)
    cst = sb("cst_s", [128, 5, 128], F32)
    cstb = sb("cstb", [128, 5, 128], BF16)
    ones_b = sb("ones_b", [128, 128], BF16)
    ones_f = sb("ones_f", [128, 128], F32)
    epsc = sb("epsc", [128, 1], F32)
    onec = sb("onec", [128, 1], F32)
    sel65 = sb("sel65", [128, 64], F32)
    w64b = sb("w64b", [128, 64], BF16)
    hmask = sb("hmask", [128, 2], F32)
    S32 = [sb("S32_%d" % i, [128, 128], F32) for i in range(2)]
    Sb = [sb("Sb_%d" % i, [128, 128], BF16) for i in range(2)]
    rstd = sb("rstd", [128, 512], F32)
    rn = sb("rn_f", [128, 512], F32)
    sqbf = sb("sqbf", [128, 512], BF16)
    NI = 2
    gi = []
    for i in range(NI):
        d = {}
        for nm in ["X", "XT", "Xn", "XTn", "Pm", "Pn", "IT", "kgc", "kg0", "kg1", "vtok", "qgT", "nwT", "vnew"]:
            d[nm] = sb("gi%d_%s" % (i, nm), [128, 128], BF16)
        for nm in ["R", "LT", "EB", "LTs", "LTi"]:
            d[nm] = sb("gi%d_%s" % (i, nm), [128, 128], F32)
        d["sc"] = sb("gi%d_sc" % i, [128, 8], F32)
        gi.append(d)

    ident_f, tri_f, triblk_f, strict_f, onesblk_f = [cst[:, i, :] for i in range(5)]
    ident_b, tri_b = cstb[:, 0, :], cstb[:, 1, :]

    pp = [P.ps("pp%d" % i, [128, 512], F32) for i in range(2)]
    pst = [P.ps("pst%d" % i, [128, 512], F32) for i in range(2)]
    pot = [P.ps("pot%d" % i, [128, 512], F32) for i in range(2)]
    pg_t = P.ps("pg", [128, 512], F32)
    pgb_t = P.ps("pgb", [128, 1024], BF16)

    class Rg:
        def __init__(self, ap, name, dep=None):
            self.ap = ap
            self.d = dep if dep is not None else Dep(name)

        def __getitem__(self, idx):
            return self.ap[idx]

    pg = [Rg(t_[:, 0:128], "pgv", t_.d) for t_ in (pg_t, pot[1])]
    pgb = [Rg(pgb_t[:, i * 128:(i + 1) * 128], "pgb%d" % i, pgb_t.d) for i in range(8)]
    pov = [Rg(pot[1][:, 128 + 128 * i:256 + 128 * i], "pov", pot[1].d) for i in range(2)]
    P._deps_orig = P._deps

    def _deps(lst):
        out = []
        for x in lst:
            if x is None:
                continue
            if isinstance(x, (T, Rg)):
                out.append(x.d)
            else:
                out.append(x)
        return out
    P._deps = _deps

    cnt = {}

    def nxt(key, n=2):
        v = cnt.get(key, 0)
        cnt[key] = v + 1
        return v % n

    def mm(out, lhsT, rhs, start, stop, reads, writes):
        P.op("pe", lambda e: e.matmul(out, lhsT=lhsT, rhs=rhs, start=start, stop=stop), reads=reads, writes=writes)

    def tr(out, in_, idn, reads, writes):
        P.op("pe", lambda e: e.transpose(out, in_, idn), reads=reads, writes=writes)

    def act(out, in_, func, reads, writes, bias=None, scale=1.0):
        if bias is None:
            P.op("act", lambda e: e.activation(out, in_, func, scale=scale), reads=reads, writes=writes)
        else:
            P.op("act", lambda e: e.activation(out, in_, func, bias=bias, scale=scale), reads=reads, writes=writes)

    def ts(out, in0, s1, s2, op0, op1, reads, writes, eng="dve"):
        if op1 is None:
            P.op(eng, lambda e: e.tensor_scalar(out, in0, s1, None, op0), reads=reads, writes=writes)
        else:
            P.op(eng, lambda e: e.tensor_scalar(out, in0, s1, s2, op0, op1), reads=reads, writes=writes)

    def tt(out, in0, in1, op, reads, writes, eng="dve"):
        P.op(eng, lambda e: e.tensor_tensor(out, in0, in1, op), reads=reads, writes=writes)

    def stt(out, in0, sc, in1, op0, op1, reads, writes):
        P.op("dve", lambda e: e.scalar_tensor_tensor(out, in0, sc, in1, op0, op1), reads=reads, writes=writes)

    def cp(out, in_, reads, writes, eng="dve"):
        if eng == "act":
            P.op("act", lambda e: e.copy(out, in_), reads=reads, writes=writes)
        else:
            P.op(eng, lambda e: e.tensor_copy(out, in_), reads=reads, writes=writes)

    def ms(t, ap, val, eng="dve"):
        P.op(eng, lambda e: e.memset(ap, val), writes=[t])

    for t_, d_ in [(g1s, g1_d), (fbs, fb_d), (gfoxs, gfox_d), (cws, cw_d), (nA, alog_d), (dtbs, dtb_d), (ggdns, ggdn_d)]:
        P.dma("sp", lambda e, t_=t_, d_=d_: e.dma_start(out=t_[:], in_=d_), writes=[t_])
    P.dma("sp", lambda e: e.dma_start(out=cst[:], in_=cst_d), writes=[cst])
    P.dma("pool", lambda e: e.dma_start(out=cstb[:], in_=cst_d), writes=[cstb])
    for k in range(KD):
        P.dma("pool", lambda e, k=k: e.dma_start(out=WAb[:, k, :], in_=WA_d[:, k, :]), writes=[WAb])
    ms(ones_b, ones_b[:], 1.0)
    ms(ones_f, ones_f[:], 1.0)
    ms(epsc, epsc[:], EPS)
    ms(onec, onec[:], 1.0)
    ms(sel65, sel65[:], 0.0)
    ms(sel65, sel65[64:65, :], 1.0)
    ms(w64b, w64b[:], 1.0)
    ms(hmask, hmask[:], 0.0)
    ms(hmask, hmask[0:64, 0:1], 1.0)
    ms(hmask, hmask[64:128, 1:2], 1.0)
    ms(QA, QA[:], 0.0, "pool")
    ms(QB, QB[:], 0.0, "pool")
    ms(VA, VA[:], 1.0, "pool")
    ms(CV, CV[:], 0.0, "pool")
    ms(carry, carry[:], 0.0)
    for i in range(2):
        ms(S32[i], S32[i][:], 0.0)
        ms(Sb[i], Sb[i][:], 0.0)
    for i in range(NI):
        ms(gi[i]["vnew"], gi[i]["vnew"][:], 0.0, "pool")
    act(nA[:], nA[:], AF.Exp, [nA], [nA])
    ts(nA[:], nA[:], -1.0, None, ALU.mult, None, [nA], [nA])

    def s1_load(jn):
        P.dma("sp", lambda e, jn=jn: e.dma_start(out=xt[:], in_=xT_d[:, :, jn * 512:(jn + 1) * 512]), writes=[xt])

    def s2_norm():
        pmt = pp[nxt("pp")]
        for k in range(KD):
            s = sqb[nxt("sqb")]
            act(s[:], xt[:, k, :], AF.Square, [xt], [s])
            mm(pmt[:], ones_b[:], s[:], k == 0, k == KD - 1, [s, ones_b], [pmt])
        act(rstd[:], pmt[:], AF.Ln, [pmt, epsc], [rstd], bias=epsc[:], scale=1.0 / D)
        act(rstd[:], rstd[:], AF.Exp, [rstd], [rstd], scale=-0.5)
        for k in range(KD):
            stt(hT[:, k, :], xt[:, k, :], g1s[:, k:k + 1], rstd[:], ALU.mult, ALU.mult, [xt, g1s, rstd], [hT])

    for j in range(NT):
        t0 = j * 512
        if j == 0:
            s1_load(0)
            s2_norm()
        for grp in range(12):
            p = pp[nxt("pp")]
            for k in range(KD):
                mm(p[:], WAb[:, k, grp * 128:(grp + 1) * 128], hT[:, k, :], k == 0, k == KD - 1, [WAb, hT], [p])
            if grp < 2:
                cp(QA[0:64, grp, :], p[0:64, :], [p], [QA], "act")
                cp(QB[64:128, grp, :], p[64:128, :], [p], [QB], "act")
            elif grp < 4:
                cp(KT[:, grp - 2, t0:t0 + 512], p[:], [p], [KT], "act")
            elif grp < 10:
                cp(CV[:, grp - 4, 3:515], p[:], [p], [CV], "dve")
            else:
                act(gate[:, grp - 10, :], p[:], AF.Silu, [p], [gate])
        for s_ in range(4):
            p = pp[nxt("pp")]
            for k in range(KD):
                mm(p[:, 0:TMC], hT[:, k, s_ * 128:(s_ + 1) * 128], WAb[:, k, 1536:1536 + TMC], k == 0, k == KD - 1, [WAb, hT], [p])
            kt = 4 * j + s_
            for h4 in range(4):
                cp(VA[:, kt, h4, 0:64], p[:, h4 * 64:(h4 + 1) * 64], [p], [VA], "dve")
            cp(SM[:, s_, :], p[:, 256:264], [p], [SM], "dve")
            tt(tf[:, 0:4], SM[:, s_, 0:4], fbs[:], ALU.add, [SM, fbs], [tf])
            act(tf[:, 0:4], tf[:, 0:4], AF.Exp, [tf], [tf], scale=-1.0)
            act(tf[:, 0:4], tf[:, 0:4], AF.Ln, [tf, onec], [tf], bias=onec[:])
            pq = pg[nxt("pg", 2)]
            mm(pq[:, 0:4], tri_f, tf[:, 0:4], True, True, [cst, tf], [pq])
            mm(pq[:, 4:8], ones_f[:], tf[:, 0:4], True, True, [ones_f, tf], [pq])
            tt(cK[:, kt, :], pq[:, 0:4], carry[:], ALU.add, [pq, carry], [cK])
            tt(carry[:], pq[:, 4:8], carry[:], ALU.add, [pq, carry], [carry])
            if s_ == 1:
                cp(crefb[:], carry[:], [carry], [crefb])
            tt(tf[:, 4:6], SM[:, s_, 4:6], dtbs[:], ALU.add, [SM, dtbs], [tf])
            act(tf[:, 4:6], tf[:, 4:6], AF.Exp, [tf], [tf])
            act(tf[:, 4:6], tf[:, 4:6], AF.Ln, [tf, onec], [tf], bias=onec[:])
            tt(GB[:, s_, 0:2], tf[:, 4:6], nA[:], ALU.mult, [tf, nA], [GB])
            act(GB[:, s_, 2:4], SM[:, s_, 6:8], AF.Sigmoid, [SM], [GB])
            ts(GB[:, s_, 4:6], GB[:, s_, 2:4], -1.0, None, ALU.mult, None, [GB], [GB])

        def fox_gen(j=j, t0=t0):
            nkt = 4 * j + 4
            pending = []
            for h in range(4):
                pr, hp = h // 2, h % 2
                Qs = QA if hp == 0 else QB
                ts(biasJ[:, h, 0:nkt], cK[:, 0:nkt, h], crefb[:, h:h + 1], None, ALU.subtract, None, [cK, crefb], [biasJ])
                po = pot[0]

                def qk(kt):
                    dg_ = kt - 4 * j
                    qlo = 128 * dg_ if dg_ > 0 else 0
                    ps_ = pst[kt % 2]
                    mm(ps_[:, qlo:512], KT[:, pr, kt * 128:(kt + 1) * 128], Qs[:, pr, qlo:512], True, True, [KT, Qs], [ps_])
                    return ps_, qlo, dg_
                pend = {0: qk(0)}
                if nkt > 1:
                    pend[1] = qk(1)
                for kt in range(nkt):
                    ps_, qlo, dg_ = pend.pop(kt)
                    pt = PT[nxt("PT", 3)]
                    act(pt[:, qlo:512], ps_[:, qlo:512], AF.Exp, [ps_, biasJ], [pt], bias=biasJ[:, h, kt:kt + 1], scale=0.125)
                    if dg_ >= 0:
                        tt(pt[:, qlo:qlo + 128], pt[:, qlo:qlo + 128], tri_b, ALU.mult, [pt, cstb], [pt], eng="pool")
                    if kt + 2 < nkt:
                        pend[kt + 2] = qk(kt + 2)
                    mm(po[0:65, qlo:512], VA[:, kt, h, :], pt[:, qlo:512], kt == 0, kt == nkt - 1, [VA, pt], [po])
                    if pending and kt == min(2, nkt - 2):
                        pending.pop(0)()
                    yield
                cp(sqf[0:65, :], po[0:65, :], [po], [sqf], "act")

                def post_rest(h=h):
                    pn = pp[0]
                    mm(pn[0:64, :], sel65[0:65, :], sqf[0:65, :], True, True, [sel65, sqf], [pn])
                    P.op("dve", lambda e, pn=pn: e.reciprocal(rn[0:64, :], pn[0:64, :]), reads=[pn], writes=[rn])
                    tt(sqf[0:64, :], sqf[0:64, :], rn[0:64, :], ALU.mult, [sqf, rn], [sqf])
                    act(sqbf[0:64, :], sqf[0:64, :], AF.Square, [sqf], [sqbf])
                    mm(pn[0:64, :], w64b[0:64, :], sqbf[0:64, :], True, True, [w64b, sqbf], [pn])
                    act(rn[0:64, :], pn[0:64, :], AF.Ln, [pn, epsc], [rn], bias=epsc[0:64, :], scale=1.0 / 64)
                    act(rn[0:64, :], rn[0:64, :], AF.Exp, [rn], [rn], scale=-0.5)
                    fs = fo_st[nxt("fo")]
                    stt(fs[0:64, :], sqf[0:64, :], gfoxs[:, h:h + 1], rn[0:64, :], ALU.mult, ALU.mult, [sqf, gfoxs, rn], [fs])
                    P.dma("sp", lambda e, fs=fs, h=h, t0=t0: e.dma_start(out=mixT_d[h * 64:(h + 1) * 64, t0:t0 + 512], in_=fs[0:64, :]), reads=[fs])
                pending.append(post_rest)
                yield
            while pending:
                pending.pop(0)()
                yield

        def gdn_gen(j=j, t0=t0):
            for i in range(6):
                c_ = cvt[0]
                ts(c_[:], CV[:, i, 0:512], cws[:, i * 4:i * 4 + 1], None, ALU.mult, None, [CV, cws], [c_])
                for k in range(1, 4):
                    stt(c_[:], CV[:, i, k:k + 512], cws[:, i * 4 + k:i * 4 + k + 1], c_[:], ALU.mult, ALU.add, [CV, cws, c_], [c_])
                if i % 3 == 2:
                    act(qkvn[:, i, :], c_[:], AF.Silu, [c_], [qkvn])
                else:
                    s_ = cvs[0]
                    act(s_[:], c_[:], AF.Silu, [c_], [s_])
                    q_ = sqb[nxt("sqb")]
                    act(q_[:], s_[:], AF.Square, [s_], [q_])
                    p = pp[1]
                    mm(p[:], ones_b[:], q_[:], True, True, [ones_b, q_], [p])
                    act(rstd[:], p[:], AF.Ln, [p, epsc], [rstd], bias=epsc[:])
                    act(rstd[:], rstd[:], AF.Exp, [rstd], [rstd], scale=-0.5)
                    stt(qkvn[:, i, :], s_[:], (128 ** -0.5) if i % 3 == 0 else 1.0, rstd[:], ALU.mult, ALU.mult, [s_, rstd], [qkvn])
                yield
            cp(CV[:, :, 0:3], CV[:, :, 512:515], [CV], [CV])

            for s_ in range(4):
                c0 = s_ * 128
                cs_ = slice(c0, c0 + 128)
                for hd in range(2):
                    G = gi[hd]
                    qT, kT, vT = qkvn[:, 3 * hd, cs_], qkvn[:, 3 * hd + 1, cs_], qkvn[:, 3 * hd + 2, cs_]
                    g_ = GB[:, s_, hd:hd + 1]
                    beta = GB[:, s_, 2 + hd:3 + hd]
                    nbeta = GB[:, s_, 4 + hd:5 + hd]
                    sc = G["sc"]
                    pq = pg[nxt("pg", 2)]
                    mm(pq[:, 0:1], triblk_f, g_, True, True, [cst, GB], [pq])
                    mm(pq[:, 1:2], onesblk_f, g_, True, True, [cst, GB], [pq])
                    cp(sc[:, 0:2], pq[:, 0:2], [pq], [sc])
                    ts(G["R"][:], triblk_f, g_, None, ALU.mult, None, [cst, GB], [G["R"]])
                    pq = pg[nxt("pg", 2)]
                    mm(pq[:], ones_f[:], G["R"][:], True, True, [ones_f, G["R"]], [pq])
                    ts(G["LT"][:], pq[:], sc[:, 0:1], 0.0, ALU.subtract, ALU.min, [pq, sc], [G["LT"]])
                    act(G["LT"][:], G["LT"][:], AF.Exp, [G["LT"]], [G["LT"]])
                    act(G["EB"][:], pq[:], AF.Exp, [pq], [G["EB"]])
                    tt(G["LTs"][:], G["LT"][:], strict_f, ALU.mult, [G["LT"], cst], [G["LTs"]], eng="pool")
                    tt(G["LTi"][:], G["LT"][:], triblk_f, ALU.mult, [G["LT"], cst], [G["LTi"]], eng="pool")
                    act(sc[:, 2:3], sc[:, 0:1], AF.Exp, [sc], [sc])
                    act(sc[:, 3:4], sc[:, 0:1], AF.Exp, [sc], [sc], bias=sc[:, 1:2], scale=-1.0)
                    ts(sc[:, 4:6], hmask[:], sc[:, 3:4], None, ALU.mult, None, [hmask, sc], [sc])
                    pk = pgb[nxt("pgb", 8)]
                    tr(pk[:], kT, ident_b, [qkvn, cstb], [pk])
                    ts(G["kgc"][:], pk[:], sc[:, 2:3], None, ALU.mult, None, [pk, sc], [G["kgc"]])
                    ts(G["kg0"][:], pk[:], sc[:, 4:5], None, ALU.mult, None, [pk, sc], [G["kg0"]])
                    ts(G["kg1"][:], pk[:], sc[:, 5:6], None, ALU.mult, None, [pk, sc], [G["kg1"]])
                    pv = pgb[nxt("pgb", 8)]
                    tr(pv[:], vT, ident_b, [qkvn, cstb], [pv])
                    cp(G["vtok"][:], pv[:], [pv], [G["vtok"]], "dve")
                    pq = pg[nxt("pg", 2)]
                    mm(pq[:], kT, kT, True, True, [qkvn], [pq])
                    stt(G["X"][:], pq[:], nbeta, G["LTs"][:], ALU.mult, ALU.mult, [pq, GB, G["LTs"]], [G["X"]])
                    pq = pg[nxt("pg", 2)]
                    mm(pq[:], kT, qT, True, True, [qkvn], [pq])
                    tt(G["IT"][:], pq[:], G["LTi"][:], ALU.mult, [pq, G["LTi"]], [G["IT"]])
                    tt(G["qgT"][:], qT, G["EB"][:], ALU.mult, [qkvn, G["EB"]], [G["qgT"]])
                    pk = pgb[nxt("pgb", 8)]
                    tr(pk[:], G["X"][:], ident_b, [G["X"], cstb], [pk])
                    cp(G["XT"][:], pk[:], [pk], [G["XT"]], "dve")
                    tt(G["Pm"][:], G["X"][:], ident_b, ALU.add, [G["X"], cstb], [G["Pm"]])
                    yield
                X = ["X", "X"]
                XTn_ = ["XT", "XT"]
                Pn_ = ["Pm", "Pm"]
                for lvl in range(5):
                    for hd in range(2):
                        G = gi[hd]
                        xa, xta = G[X[hd]], G[XTn_[hd]]
                        xb_n, xtb_n = ("Xn", "XTn") if X[hd] == "X" else ("X", "XT")
                        xb, xtb = G[xb_n], G[xtb_n]
                        pa_n = Pn_[hd]
                        pb_n = "Pn" if pa_n == "Pm" else "Pm"
                        pq = pg[nxt("pg", 2)]
                        mm(pq[:], xta[:], xa[:], True, True, [xta, xa], [pq])
                        cp(xb[:], pq[:], [pq], [xb], "dve")
                        pq2 = pg[nxt("pg", 2)]
                        mm(pq2[:], xa[:], xta[:], True, True, [xta, xa], [pq2])
                        cp(xtb[:], pq2[:], [pq2], [xtb], "dve")
                        pq3 = pg[nxt("pg", 2)]
                        mm(pq3[:], xtb[:], G[pa_n][:], True, True, [xtb, G[pa_n]], [pq3])
                        tt(G[pb_n][:], pq3[:], G[pa_n][:], ALU.add, [pq3, G[pa_n]], [G[pb_n]])
                        X[hd], XTn_[hd], Pn_[hd] = xb_n, xtb_n, pb_n
                        yield
                for hd in range(2):
                    G = gi[hd]
                    Pf = G[Pn_[hd]]
                    pq = pg[nxt("pg", 2)]
                    mm(pq[:], G["kgc"][:], Pf[:], True, True, [G["kgc"], Pf], [pq])
                    ts(G["nwT"][:], pq[:], -1.0, None, ALU.mult, None, [pq], [G["nwT"]])
                po_ = [pov[0], pov[1]]
                for X_ in range(2):
                    rs_ = slice(64 * X_, 64 * X_ + 64)
                    for hd in range(2):
                        G = gi[hd]
                        Pf = G[Pn_[hd]]
                        beta = GB[:, s_, 2 + hd:3 + hd]
                        pq = pg[nxt("pg", 2)]
                        mm(pq[:], Pf[:], G["vtok"][:], True, False, [Pf, G["vtok"]], [pq])
                        mm(pq[:], G["nwT"][:], Sb[hd][:], False, True, [G["nwT"], Sb[hd]], [pq])
                        ts(G["vnew"][rs_, :], pq[rs_, :], GB[rs_, s_, 2 + hd:3 + hd], None, ALU.mult, None, [pq, GB], [G["vnew"]])
                        pq2 = po_[hd]
                        mm(pq2[:, rs_], Sb[hd][:], G["qgT"][:, rs_], True, False, [Sb[hd], G["qgT"]], [pq2])
                        mm(pq2[:, rs_], G["vnew"][:], G["IT"][:, rs_], False, True, [G["vnew"], G["IT"]], [pq2])
                        pq3 = pg[nxt("pg", 2)]
                        kgx = G["kg0"] if X_ == 0 else G["kg1"]
                        mm(pq3[:], kgx[:], G["vnew"][:], True, True, [kgx, G["vnew"]], [pq3])
                        stt(S32[hd][:], S32[hd][:], G["EB"][:, 64 * X_ + 63:64 * X_ + 64], pq3[:], ALU.mult, ALU.add, [S32[hd], G["EB"], pq3], [S32[hd]])
                        cp(Sb[hd][:], S32[hd][:], [S32[hd]], [Sb[hd]], "dve")
                        yield
                for hd in range(2):
                    cp(OT[hd][:, cs_], po_[hd][:], [po_[hd]], [OT[hd]], "dve")
            for hd in range(2):
                q_ = sqb[nxt("sqb")]
                act(q_[:], OT[hd][:], AF.Square, [OT[hd]], [q_])
                p = pp[1]
                mm(p[:], ones_b[:], q_[:], True, True, [ones_b, q_], [p])
                act(rstd[:], p[:], AF.Ln, [p, epsc], [rstd], bias=epsc[:], scale=1.0 / 128)
                act(rstd[:], rstd[:], AF.Exp, [rstd], [rstd], scale=-0.5)
                gs = OT[hd]
                stt(gs[:], OT[hd][:], ggdns[:, 0:1], rstd[:], ALU.mult, ALU.mult, [OT[hd], ggdns, rstd], [gs])
                tt(gs[:], gs[:], gate[:, hd, :], ALU.mult, [gs, gate], [gs])
                P.dma("sp", lambda e, gs=gs, hd=hd, t0=t0: e.dma_start(out=mixT_d[256 + hd * 128:256 + (hd + 1) * 128, t0:t0 + 512], in_=gs[:]), reads=[gs])

        gf_, gg_ = fox_gen(), gdn_gen()
        n_f, n_g = 4 * (4 * j + 4 + 1), 70
        acc = 0.0
        alive_f, alive_g = True, True
        gsteps = 0
        if j + 1 < NT:
            s1_load(j + 1)
        while alive_f or alive_g:
            if alive_g:
                try:
                    next(gg_)
                    gsteps += 1
                    if gsteps == 24 and j + 1 < NT:
                        s2_norm()
                except StopIteration:
                    alive_g = False
            acc += n_f / n_g
            while alive_f and (acc >= 1.0 or not alive_g):
                acc -= 1.0
                try:
                    next(gf_)
                except StopIteration:
                    alive_f = False
    evs = []
    for t_ in fo_st + OT:
        evs += list(t_.d.rs)
    P.finish("sp", evs)
    P.emit()
    return nc


D = 1024
KD = 8
EPS = 1e-6


def build_B(NTOK, TG, E, F, moe, final):
    nc = bass.Bass("TRN2", target_bir_lowering=False)
    P = Prog(nc)
    NF = F // 128
    NTT = TG // 512
    NG = NTOK // TG
    dt = nc.dram_tensor
    xT_d = dt("xT", [D, NTOK], F32, kind="ExternalInput").ap().rearrange("(k p) n -> p k n", p=128)
    mixT_d = dt("mixT", [D, NTOK], F32, kind="ExternalInput").ap().rearrange("(k p) n -> p k n", p=128)
    wout_d = dt("wout", [D, D], F32, kind="ExternalInput").ap().rearrange("(k p) n -> p k n", p=128)
    g2_d = dt("g2", [128, KD], F32, kind="ExternalInput").ap()
    gf_d = dt("gf", [128, KD], F32, kind="ExternalInput").ap()
    w1_d = dt("w1", [E, D, F], F32, kind="ExternalInput").ap()
    w3_d = dt("w3", [E, D, F], F32, kind="ExternalInput").ap()
    w2_d = dt("w2", [E, F, D], F32, kind="ExternalInput").ap()
    wr_d = dt("wr", [D, 8], F32, kind="ExternalInput").ap().rearrange("(k p) n -> p k n", p=128)
    id_d = dt("ident", [128, 128], F32, kind="ExternalInput").ap()
    yT_d = dt("yT", [D, NTOK], F32, kind="ExternalOutput").ap().rearrange("(k p) n -> p k n", p=128)
    if final:
        yfT_d = dt("yfT", [D, NTOK], F32, kind="ExternalOutput").ap().rearrange("(k p) n -> p k n", p=128)

    xs = P.sb("xs", [128, KD, TG], F32)
    hT = P.sb("hT", [128, KD, TG], BF16)
    gT = P.sb("gT", [128, NF, TG], BF16)
    mixs = gT
    woutb = P.sb("woutb", [128, KD, D], BF16)
    w1b = [P.sb("w1b%d" % i, [128, KD, 256], BF16) for i in range(2)]
    w3b = [P.sb("w3b%d" % i, [128, KD, 256], BF16) for i in range(2)]
    w2b = [P.sb("w2b%d" % i, [128, NF, 128], BF16) for i in range(2)]
    g2s = P.sb("g2s", [128, KD], F32)
    gfs = P.sb("gfs", [128, KD], F32)
    ident = P.sb("ident_s", [128, 128], F32)
    ones_b = P.sb("ones_b", [128, 128], BF16)
    ones_f = P.sb("ones_f", [128, 128], F32)
    epsc = P.sb("epsc", [128, 1], F32)
    sq = [P.sb("sq%d" % i, [128, 512], BF16) for i in range(2)]
    rstd = P.sb("rstd", [128, 512], F32)
    sil = [P.sb("sil%d" % i, [128, 512], F32) for i in range(2)]
    tmpb = [P.sb("tmpb%d" % i, [128, 512], F32) for i in range(2)]
    if moe:
        h32 = [P.sb("h32_%d" % i, [128, 512], F32) for i in range(2)]
        wrs = P.sb("wrs", [128, KD, 8], F32)
        lg = P.sb("lg", [128, 8], F32)
        lgT = P.sb("lgT", [8, 512], F32)
        top8 = P.sb("top8", [128, 8], F32)
        rw = P.sb("rw", [128, TG // 128, 8], F32)
        tmp8 = P.sb("tmp8", [128, 8], F32)
        tmp1 = P.sb("tmp1", [128, 2], F32)
        dg = [P.sb("dg%d" % i, [128, 128], F32) for i in range(2)]
        wbc = P.sb("wbc", [128, TG], F32)
    pa = [P.ps("pa%d" % i, [128, 512], F32) for i in range(2)]
    pb = [P.ps("pb%d" % i, [128, 512], F32) for i in range(2)]
    py = [P.ps("py%d" % i, [128, 512], F32) for i in range(2)]
    pm = [P.ps("pm%d" % i, [128, 512], F32) for i in range(2)]

    P.dma("sp", lambda e: e.dma_start(out=ident[:], in_=id_d), writes=[ident])
    P.dma("sp", lambda e: e.dma_start(out=g2s[:], in_=g2_d), writes=[g2s])
    P.dma("sp", lambda e: e.dma_start(out=gfs[:], in_=gf_d), writes=[gfs])
    P.op("dve", lambda e: e.memset(ones_b[:], 1.0), writes=[ones_b])
    P.op("dve", lambda e: e.memset(ones_f[:], 1.0), writes=[ones_f])
    P.op("dve", lambda e: e.memset(epsc[:], EPS), writes=[epsc])
    for k in range(KD):
        P.dma("pool", lambda e, k=k: e.dma_start(out=woutb[:, k, :], in_=wout_d[:, k, :]), writes=[woutb])
    if moe:
        P.dma("sp", lambda e: e.dma_start(out=wrs[:], in_=wr_d), writes=[wrs])

    cnt = {"sq": 0, "pm": 0, "pa": 0, "py": 0, "w13": 0, "w2": 0, "sil": 0, "dg": 0, "h32": 0}

    def nxt(key, n=2):
        v = cnt[key] % n
        cnt[key] += 1
        return v

    def rmsnorm(src, gs, dst_bf, tt, dst32=None):
        sl = slice(tt * 512, (tt + 1) * 512)
        pmt = pm[nxt("pm")]
        for k in range(KD):
            s = sq[nxt("sq")]
            P.op("act", lambda e, s=s, k=k: e.activation(s[:], src[:, k, sl], AF.Square), reads=[src], writes=[s])
            P.op("pe", lambda e, s=s, k=k: e.matmul(pmt[:], lhsT=ones_b[:], rhs=s[:], start=(k == 0), stop=(k == KD - 1)),
                 reads=[s, ones_b], writes=[pmt])
        P.op("act", lambda e: e.activation(rstd[:], pmt[:], AF.Sqrt, bias=epsc[:], scale=1.0 / D), reads=[pmt, epsc], writes=[rstd])
        P.op("dve", lambda e: e.reciprocal(rstd[:], rstd[:]), reads=[rstd], writes=[rstd])
        for k in range(KD):
            P.op("dve", lambda e, k=k: e.scalar_tensor_tensor(dst_bf[:, k, sl], src[:, k, sl], gs[:, k:k + 1], rstd[:], ALU.mult, ALU.mult),
                 reads=[src, gs, rstd], writes=[dst_bf])

    for gi in range(NG):
        t0 = gi * TG
        P.dma("sp", lambda e, t0=t0: e.dma_start(out=xs[:], in_=xT_d[:, :, t0:t0 + TG]), writes=[xs])
        for k in range(KD):
            P.dma("pool", lambda e, t0=t0, k=k: e.dma_start(out=mixs[:, k, :], in_=mixT_d[:, k, t0:t0 + TG]), writes=[mixs])
        for g in range(KD):
            for tt in range(NTT):
                sl = slice(tt * 512, (tt + 1) * 512)
                p = py[nxt("py")]
                for k in range(KD):
                    P.op("pe", lambda e, p=p, k=k, g=g, sl=sl: e.matmul(p[:], lhsT=woutb[:, k, g * 128:(g + 1) * 128], rhs=mixs[:, k, sl],
                                                                     start=(k == 0), stop=(k == KD - 1)), reads=[woutb, mixs], writes=[p])
                P.op("dve", lambda e, p=p, g=g, sl=sl: e.tensor_tensor(xs[:, g, sl], xs[:, g, sl], p[:], ALU.add), reads=[xs, p], writes=[xs])
        for tt in range(NTT):
            rmsnorm(xs, g2s, hT, tt)
            if moe:
                sl_ = slice(tt * 512, (tt + 1) * 512)
                prt = pm[nxt("pm")]
                for k in range(KD):
                    hk = h32[nxt("h32")]
                    P.op("dve", lambda e, k=k, hk=hk, sl_=sl_: e.scalar_tensor_tensor(hk[:], xs[:, k, sl_], g2s[:, k:k + 1], rstd[:], ALU.mult, ALU.mult),
                         reads=[xs, g2s, rstd], writes=[hk])
                    P.op("pe", lambda e, k=k, hk=hk, prt=prt: e.matmul(prt[0:8, :], lhsT=wrs[:, k, :], rhs=hk[:], start=(k == 0), stop=(k == KD - 1)),
                         reads=[hk, wrs], writes=[prt])
                P.op("act", lambda e, prt=prt: e.copy(lgT[:], prt[0:8, :]), reads=[prt], writes=[lgT])
                prt = pm[nxt("pm")]
                for ts in range(4):
                    P.op("pe", lambda e, ts=ts, prt=prt: e.transpose(prt[:, ts * 8:(ts + 1) * 8], lgT[0:8, ts * 128:(ts + 1) * 128], ident[0:8, 0:8]),
                         reads=[lgT, ident], writes=[prt])
                for ts in range(4):
                    st = tt * 4 + ts
                    p = prt
                    P.op("dve", lambda e, p=p, ts=ts: e.tensor_copy(lg[:], p[:, ts * 8:(ts + 1) * 8]), reads=[p], writes=[lg])
                    P.op("dve", lambda e: e.max(out=top8[:], in_=lg[:]), reads=[lg], writes=[top8])
                    P.op("dve", lambda e: e.tensor_scalar(tmp1[:, 0:1], top8[:, 0:1], -1.0, None, ALU.mult), reads=[top8], writes=[tmp1])
                    P.op("act", lambda e: e.activation(tmp8[:], lg[:], AF.Exp, bias=tmp1[:, 0:1], scale=1.0), reads=[lg, tmp1], writes=[tmp8])
                    P.op("act", lambda e: e.activation(tmp1[:, 1:2], top8[:, 1:2], AF.Exp, bias=tmp1[:, 0:1], scale=1.0), reads=[top8, tmp1], writes=[tmp1])
                    P.op("dve", lambda e: e.tensor_scalar(tmp1[:, 1:2], tmp1[:, 1:2], 1.0, None, ALU.add), reads=[tmp1], writes=[tmp1])
                    P.op("dve", lambda e: e.reciprocal(tmp1[:, 1:2], tmp1[:, 1:2]), reads=[tmp1], writes=[tmp1])
                    P.op("dve", lambda e: e.tensor_scalar(lg[:], lg[:], top8[:, 1:2], None, ALU.is_ge), reads=[lg, top8], writes=[lg])
                    P.op("dve", lambda e, st=st: e.scalar_tensor_tensor(rw[:, st, :], tmp8[:], tmp1[:, 1:2], lg[:], ALU.mult, ALU.mult),
                         reads=[tmp8, tmp1, lg], writes=[rw])
        tasks = []
        for ex in range(E):
            for fp in range(NF // 2):
                tasks.append(("h", ex, fp))
            for g in range(KD):
                tasks.append(("y", ex, g))
        bufs = {}

        def issue(i):
            kind, ex, j = tasks[i]
            if kind == "h":
                wi = nxt("w13")
                a_, b_ = w1b[wi], w3b[wi]
                bufs[i] = (a_, b_)
                f0 = j * 256
                src1 = w1_d[ex].rearrange("(k p) f -> p k f", p=128)
                src3 = w3_d[ex].rearrange("(k p) f -> p k f", p=128)
                for k in range(KD):
                    P.dma("pool", lambda e, a_=a_, k=k, f0=f0, src1=src1: e.dma_start(out=a_[:, k, :], in_=src1[:, k, f0:f0 + 256]), writes=[a_])
                    P.dma("pool", lambda e, b_=b_, k=k, f0=f0, src3=src3: e.dma_start(out=b_[:, k, :], in_=src3[:, k, f0:f0 + 256]), writes=[b_])
            else:
                w_ = w2b[nxt("w2")]
                bufs[i] = w_
                src2 = w2_d[ex].rearrange("(c p) d -> p c d", p=128)
                P.dma("pool", lambda e, w_=w_, j=j, src2=src2: e.dma_start(out=w_[:], in_=src2[:, :, j * 128:(j + 1) * 128]), writes=[w_])

        issue(0)
        for i, (kind, ex, j) in enumerate(tasks):
            if i + 1 < len(tasks):
                issue(i + 1)
            if kind == "h" and j == 0 and moe:
                for st in range(TG // 128):
                    d_ = dg[nxt("dg")]
                    P.op("dve", lambda e, d_=d_, st=st, ex=ex: e.tensor_scalar(d_[:], ident[:], rw[:, st, ex:ex + 1], None, ALU.mult),
                         reads=[ident, rw], writes=[d_])
                    p = pm[nxt("pm")]
                    P.op("pe", lambda e, p=p, d_=d_: e.matmul(p[:, 0:128], lhsT=ones_f[:], rhs=d_[:], start=True, stop=True), reads=[ones_f, d_], writes=[p])
                    P.op("act", lambda e, p=p, st=st: e.copy(wbc[:, st * 128:(st + 1) * 128], p[:, 0:128]), reads=[p], writes=[wbc])
            if kind == "h":
                a_, b_ = bufs[i]
                for fc in range(2):
                    f = j * 2 + fc
                    for tt in range(NTT):
                        sl = slice(tt * 512, (tt + 1) * 512)
                        pi = nxt("pa")
                        p_a, p_b = pa[pi], pb[pi]
                        for k in range(KD):
                            P.op("pe", lambda e, p_a=p_a, a_=a_, k=k, fc=fc, sl=sl: e.matmul(p_a[:], lhsT=a_[:, k, fc * 128:(fc + 1) * 128], rhs=hT[:, k, sl],
                                                                                      start=(k == 0), stop=(k == KD - 1)), reads=[a_, hT], writes=[p_a])
                        for k in range(KD):
                            P.op("pe", lambda e, p_b=p_b, b_=b_, k=k, fc=fc, sl=sl: e.matmul(p_b[:], lhsT=b_[:, k, fc * 128:(fc + 1) * 128], rhs=hT[:, k, sl],
                                                                                      start=(k == 0), stop=(k == KD - 1)), reads=[b_, hT], writes=[p_b])
                        s_ = sil[nxt("sil")]
                        P.op("act", lambda e, s_=s_, p_a=p_a: e.activation(s_[:], p_a[:], AF.Silu), reads=[p_a], writes=[s_])
                        if moe:
                            t_ = tmpb[pi]
                            P.op("dve", lambda e, t_=t_, p_b=p_b, sl=sl: e.tensor_tensor(t_[:], p_b[:], wbc[:, sl], ALU.mult), reads=[p_b, wbc], writes=[t_])
                            P.op("dve", lambda e, t_=t_, s_=s_, f=f, sl=sl: e.tensor_tensor(gT[:, f, sl], s_[:], t_[:], ALU.mult), reads=[s_, t_], writes=[gT])
                        else:
                            P.op("dve", lambda e, s_=s_, p_b=p_b, f=f, sl=sl: e.tensor_tensor(gT[:, f, sl], s_[:], p_b[:], ALU.mult), reads=[s_, p_b], writes=[gT])
            else:
                w_ = bufs[i]
                g = j
                for tt in range(NTT):
                    sl = slice(tt * 512, (tt + 1) * 512)
                    p = py[nxt("py")]
                    for f in range(NF):
                        P.op("pe", lambda e, p=p, w_=w_, f=f, sl=sl: e.matmul(p[:], lhsT=w_[:, f, :], rhs=gT[:, f, sl], start=(f == 0), stop=(f == NF - 1)),
                             reads=[w_, gT], writes=[p])
                    P.op("dve", lambda e, p=p, g=g, sl=sl: e.tensor_tensor(xs[:, g, sl], xs[:, g, sl], p[:], ALU.add), reads=[xs, p], writes=[xs])
        P.dma("sp", lambda e, t0=t0: e.dma_start(out=yT_d[:, :, t0:t0 + TG], in_=xs[:]), reads=[xs])
        if final:
            for tt in range(NTT):
                rmsnorm(xs, gfs, xs, tt)
            P.dma("sp", lambda e, t0=t0: e.dma_start(out=yfT_d[:, :, t0:t0 + TG], in_=xs[:]), reads=[xs])
    P.finish("sp", list(xs.d.rs))
    P.emit()
    return nc


from concourse.bass_utils import run_bass_kernel_spmd

SEQ = 8192
BATCH = 4
DEPTH = 4
_PROGS = {}


def _prog(key):
    if key not in _PROGS:
        if key == "A":
            _PROGS[key] = build_A(SEQ)
        elif key == "Bd":
            _PROGS[key] = build_B(SEQ // 2, 1024, 1, 2816, False, False)
        else:
            _PROGS[key] = build_B(SEQ // 2, 1024, 8, 3584, True, True)
    return _PROGS[key]


def kernel(x, ln1_g, w_in, fox_f_bias, fox_norm_g, gdn_conv_w, gdn_a_log, gdn_dt_bias,
           gdn_norm_g, w_out, ln2_g, ffn_w1, ffn_w3, ffn_w2, router_w, exp_w1, exp_w3,
           exp_w2, final_g):
    f32 = np.float32
    x = np.asarray(x, f32)
    xT = [np.ascontiguousarray(x[b].T) for b in range(BATCH)]
    ident = np.eye(128, dtype=f32)
    gf = lay8(np.asarray(final_g, f32))
    cores = list(range(8))
    H = SEQ // 2
    outT = None
    for layer in range(DEPTH):
        maps = []
        for b in range(BATCH):
            for hh in range(2):
                maps.append(prep_A(xT[b], np.asarray(ln1_g[layer], f32), np.asarray(w_in[layer], f32),
                                   np.asarray(fox_f_bias[layer], f32), np.asarray(fox_norm_g[layer], f32),
                                   np.asarray(gdn_conv_w[layer], f32), np.asarray(gdn_a_log[layer], f32),
                                   np.asarray(gdn_dt_bias[layer], f32), np.asarray(gdn_norm_g[layer], f32), hh))
        resA = run_bass_kernel_spmd(_prog("A"), maps, core_ids=cores).results
        mixT = []
        for b in range(BATCH):
            m0, m1 = np.asarray(resA[2 * b]["mixT"]), np.asarray(resA[2 * b + 1]["mixT"])
            mixT.append(np.concatenate([m0[0:256], m1[0:256], m0[256:512], m1[256:512]], axis=0))
        j = layer // 2
        moe = (layer % 2 == 1)
        maps = []
        for b in range(BATCH):
            for th in range(2):
                sl = slice(th * H, (th + 1) * H)
                m = {"xT": np.ascontiguousarray(xT[b][:, sl]), "mixT": np.ascontiguousarray(mixT[b][:, sl]),
                     "wout": np.asarray(w_out[layer], f32), "g2": lay8(np.asarray(ln2_g[layer], f32)), "gf": gf, "ident": ident}
                if moe:
                    m.update({"w1": np.asarray(exp_w1[j], f32), "w3": np.asarray(exp_w3[j], f32), "w2": np.asarray(exp_w2[j], f32),
                              "wr": np.asarray(router_w[j], f32)})
                else:
                    m.update({"w1": np.asarray(ffn_w1[j], f32)[None], "w3": np.asarray(ffn_w3[j], f32)[None],
                              "w2": np.asarray(ffn_w2[j], f32)[None], "wr": np.zeros((1024, 8), f32)})
                maps.append(m)
        resB = run_bass_kernel_spmd(_prog("Bm" if moe else "Bd"), maps, core_ids=cores).results
        for b in range(BATCH):
            xT[b] = np.concatenate([np.asarray(resB[2 * b]["yT"]), np.asarray(resB[2 * b + 1]["yT"])], axis=1)
        if layer == DEPTH - 1:
            outT = [np.concatenate([np.asarray(resB[2 * b]["yfT"]), np.asarray(resB[2 * b + 1]["yfT"])], axis=1) for b in range(BATCH)]
    out = np.stack([np.ascontiguousarray(o.T) for o in outT], axis=0).astype(f32)
    return out
```
